# Optimizing a Trainium2 kernel written in Bass

```python
import jax, jax.numpy as jnp
from jax import lax
import numpy as np

D_MODEL = 2048
BATCH = 16
SEQ = 2048
DEPTH = 4

CHUNK = 64
N_MIXERS = 3

DN_ALPHA = (2 * DEPTH) ** 0.25
DN_BETA = (8 * DEPTH) ** -0.25
LN_EPS = 1e-5

RG_WIDTH = 5 * D_MODEL // 4
RG_BLOCKS = 16
RG_BLOCK = RG_WIDTH // RG_BLOCKS
RG_C = 8.0
CONV_W = 4

SG_BLOCK = 128
SG_HALF = D_MODEL
SG_GROUPS = 8
SG_GROUP = SG_HALF // SG_GROUPS

ML_WIDTH = D_MODEL
ML_HEADS = 8
ML_HEAD = ML_WIDTH // ML_HEADS
ML_QKV_BLOCK = 4
ML_CHUNK = CHUNK

N_EXPERTS = 32
TOP_K = 4
D_EXPERT = D_MODEL // 4
SWIGLU_LIMIT = 7.0
SWIGLU_ALPHA = 1.702

N_LAYERS_A = len(range(0, DEPTH, N_MIXERS))
N_LAYERS_B = len(range(1, DEPTH, N_MIXERS))
N_LAYERS_C = len(range(2, DEPTH, N_MIXERS))

kernel_name = "hybrid_rglru_sgu_mlstm_moe_deepnorm"

F32 = jnp.float32


def layer_norm(x, g, b):
    xf = x.astype(F32)
    mu = jnp.mean(xf, axis=-1, keepdims=True)
    var = jnp.mean(jnp.square(xf - mu), axis=-1, keepdims=True)
    return ((xf - mu) * lax.rsqrt(var + LN_EPS) * g.astype(F32) + b.astype(F32)).astype(x.dtype)


def causal_dwconv(x, w, b):
    ch = x.shape[-1]
    y = lax.conv_general_dilated(
        x, w[:, None, :].astype(x.dtype), window_strides=(1,), padding=[(CONV_W - 1, 0)],
        dimension_numbers=("NWC", "WIO", "NWC"), feature_group_count=ch)
    return y + b


def blockdiag(x, w):
    nb, bs, _ = w.shape
    xb = x.reshape(x.shape[:-1] + (nb, bs))
    return jnp.einsum("...nc,ncd->...nd", xb, w).reshape(x.shape)


def linear_recurrence(a, u):
    def combine(left, right):
        a_l, u_l = left
        a_r, u_r = right
        return a_l * a_r, a_r * u_l + u_r
    _, h = lax.associative_scan(combine, (a, u), axis=1)
    return h


def rglru_mixer(h, w_in, conv_w, conv_b, w_rgate, b_rgate, w_igate, b_igate, lam, w_out):
    gate_br, rec_br = jnp.split(h @ w_in, 2, axis=-1)
    xr = causal_dwconv(rec_br, conv_w, conv_b)
    r = jax.nn.sigmoid((blockdiag(xr, w_rgate) + b_rgate).astype(F32))
    i = jax.nn.sigmoid((blockdiag(xr, w_igate) + b_igate).astype(F32))
    log_a = -RG_C * r * jax.nn.softplus(-lam.astype(F32))
    u = jnp.sqrt(-jnp.expm1(2.0 * log_a)) * (i * xr.astype(F32))
    hs = linear_recurrence(jnp.exp(log_a), u)
    return (jax.nn.gelu(gate_br) * hs.astype(h.dtype)) @ w_out


def sgu_mixer(h, w_in, b_in, ln_g, ln_b, w_sp, b_sp, w_out, b_out):
    bsz, seq, _ = h.shape
    z = jax.nn.gelu(h @ w_in + b_in)
    u, v = jnp.split(z, 2, axis=-1)
    v = layer_norm(v, ln_g, ln_b)
    vb = v.reshape(bsz, seq // SG_BLOCK, SG_BLOCK, SG_GROUPS, SG_GROUP)
    pos = jnp.arange(SG_BLOCK)
    allowed = (pos[None, :] // CHUNK) <= (pos[:, None] // CHUNK)
    w = jnp.where(allowed[None], w_sp, jnp.zeros_like(w_sp))
    mixed = jnp.einsum("gij,bnjgc->bnigc", w, vb) + b_sp.T[None, None, :, :, None]
    return (u * mixed.reshape(bsz, seq, SG_HALF)) @ w_out + b_out


def mlstm_chunkwise(q, k, v, log_i, log_f):
    _, bsz, nh, L, d = q.shape
    causal = jnp.tril(jnp.ones((L, L), dtype=bool))

    def step(carry, xs):
        C, n, m = carry
        qc, kc, vc, li, lf = xs
        b = jnp.cumsum(lf, axis=-1)
        g = b + m[..., None]
        dmat = b[..., :, None] - b[..., None, :] + li[..., None, :]
        dmat = jnp.where(causal, dmat, -jnp.inf)
        m_t = jnp.maximum(g, jnp.max(dmat, axis=-1))
        w_intra = jnp.exp(dmat - m_t[..., None])
        w_inter = jnp.exp(g - m_t)
        s = jnp.einsum("bhtd,bhsd->bhts", qc, kc) * w_intra
        num = jnp.einsum("bhts,bhsv->bhtv", s, vc) + w_inter[..., None] * jnp.einsum("bhtk,bhkv->bhtv", qc, C)
        den = jnp.sum(s, axis=-1) + w_inter * jnp.einsum("bhtk,bhk->bht", qc, n)
        out = num / jnp.maximum(jnp.abs(den), jnp.exp(-m_t))[..., None]
        b_last = b[..., -1]
        dec = b_last[..., None] - b + li
        m_new = jnp.maximum(b_last + m, jnp.max(dec, axis=-1))
        ws = jnp.exp(dec - m_new[..., None])
        wc = jnp.exp(b_last + m - m_new)
        C_new = wc[..., None, None] * C + jnp.einsum("bhs,bhsk,bhsv->bhkv", ws, kc, vc)
        n_new = wc[..., None] * n + jnp.einsum("bhs,bhsk->bhk", ws, kc)
        return (C_new, n_new, m_new), out

    init = (jnp.zeros((bsz, nh, d, d), F32), jnp.zeros((bsz, nh, d), F32), jnp.zeros((bsz, nh), F32))
    _, hs = lax.scan(step, init, (q, k, v, log_i, log_f))
    return hs


def mlstm_mixer(h, w_in, conv_w, conv_b, w_q, w_k, w_v, w_gates, b_gates, skip, norm_g, w_out):
    bsz, seq, _ = h.shape
    nc = seq // ML_CHUNK
    xm, z = jnp.split(h @ w_in, 2, axis=-1)
    xc = jax.nn.silu(causal_dwconv(xm, conv_w, conv_b))
    q = blockdiag(xc, w_q)
    k = blockdiag(xc, w_k)
    v = blockdiag(xm, w_v)
    gates = (jnp.concatenate([q, k, v], axis=-1) @ w_gates + b_gates).astype(F32)
    log_i = gates[..., :ML_HEADS]
    log_f = jax.nn.log_sigmoid(gates[..., ML_HEADS:])

    def to_chunks(t):
        return t.astype(F32).reshape(bsz, nc, ML_CHUNK, ML_HEADS, ML_HEAD).transpose(1, 0, 3, 2, 4)

    def gate_chunks(t):
        return t.reshape(bsz, nc, ML_CHUNK, ML_HEADS).transpose(1, 0, 3, 2)

    hs = mlstm_chunkwise(to_chunks(q), to_chunks(k) * (ML_HEAD ** -0.5), to_chunks(v),
                         gate_chunks(log_i), gate_chunks(log_f))
    hs = hs.transpose(1, 0, 3, 2, 4).reshape(bsz, seq, ML_HEADS, ML_HEAD)
    mu = jnp.mean(hs, axis=-1, keepdims=True)
    var = jnp.mean(jnp.square(hs - mu), axis=-1, keepdims=True)
    hn = ((hs - mu) * lax.rsqrt(var + LN_EPS)).reshape(bsz, seq, ML_WIDTH) * norm_g.astype(F32)
    hn = hn.astype(h.dtype) + skip * xc
    return (jax.nn.sigmoid(z) * hn) @ w_out


def moe_ffn(h, w_router, b_router, w_gate_up, b_gate_up, w_down, b_down):
    bsz, seq, dm = h.shape
    t = h.reshape(bsz * seq, dm)
    logits = (t @ w_router + b_router).astype(F32)
    top_val, top_idx = lax.top_k(logits, TOP_K)
    probs = jax.nn.softmax(top_val, axis=-1)
    combine = jnp.sum(jax.nn.one_hot(top_idx, N_EXPERTS, dtype=F32) * probs[..., None], axis=1)
    out = jnp.zeros((bsz * seq, dm), F32)
    for e in range(N_EXPERTS):
        gate, up = jnp.split(t @ w_gate_up[e] + b_gate_up[e], 2, axis=-1)
        gate = jnp.minimum(gate, SWIGLU_LIMIT)
        up = jnp.clip(up, -SWIGLU_LIMIT, SWIGLU_LIMIT)
        act = gate * jax.nn.sigmoid(SWIGLU_ALPHA * gate) * (up + 1)
        out = out + combine[:, e:e + 1] * (act @ w_down[e] + b_down[e]).astype(F32)
    return out.reshape(bsz, seq, dm).astype(h.dtype)


def setup_inputs(seed: int = 0) -> dict:
    key = jax.random.key(seed)
    keys = iter(jax.random.split(key, 64))
    D = D_MODEL
    H = ML_HEADS

    def nrm(shape, scale):
        return jax.random.normal(next(keys), shape, F32) * scale

    x = nrm((BATCH, SEQ, D), 1.0)
    c = nrm((BATCH, D), 1.0)
    ada_w = nrm((DEPTH, D, 6 * D), 0.1 * D ** -0.5)
    ada_b = nrm((DEPTH, 6 * D), 0.02)
    ln1_g = 1.0 + nrm((DEPTH, D), 0.02)
    ln1_b = nrm((DEPTH, D), 0.02)
    ln2_g = 1.0 + nrm((DEPTH, D), 0.02)
    ln2_b = nrm((DEPTH, D), 0.02)

    rg_w_in = nrm((N_LAYERS_A, D, 2 * RG_WIDTH), D ** -0.5)
    rg_conv_w = nrm((N_LAYERS_A, CONV_W, RG_WIDTH), CONV_W ** -0.5)
    rg_conv_b = nrm((N_LAYERS_A, RG_WIDTH), 0.02)
    rg_w_rgate = nrm((N_LAYERS_A, RG_BLOCKS, RG_BLOCK, RG_BLOCK), RG_BLOCK ** -0.5)
    rg_b_rgate = nrm((N_LAYERS_A, RG_WIDTH), 0.02)
    rg_w_igate = nrm((N_LAYERS_A, RG_BLOCKS, RG_BLOCK, RG_BLOCK), RG_BLOCK ** -0.5)
    rg_b_igate = nrm((N_LAYERS_A, RG_WIDTH), 0.02)
    a_pow = jax.random.uniform(next(keys), (N_LAYERS_A, RG_WIDTH), F32, 0.9, 0.999)
    s = a_pow ** (1.0 / RG_C)
    rg_lam = jnp.log(s) - jnp.log1p(-s)
    rg_w_out = nrm((N_LAYERS_A, RG_WIDTH, D), DN_BETA * RG_WIDTH ** -0.5)

    sg_w_in = nrm((N_LAYERS_B, D, 2 * SG_HALF), D ** -0.5)
    sg_b_in = nrm((N_LAYERS_B, 2 * SG_HALF), 0.02)
    sg_ln_g = 1.0 + nrm((N_LAYERS_B, SG_HALF), 0.02)
    sg_ln_b = nrm((N_LAYERS_B, SG_HALF), 0.02)
    sg_w_sp = nrm((N_LAYERS_B, SG_GROUPS, SG_BLOCK, SG_BLOCK), 0.5 * SG_BLOCK ** -0.5)
    sg_b_sp = 1.0 + nrm((N_LAYERS_B, SG_GROUPS, SG_BLOCK), 0.02)
    sg_w_out = nrm((N_LAYERS_B, SG_HALF, D), DN_BETA * SG_HALF ** -0.5)
    sg_b_out = nrm((N_LAYERS_B, D), 0.02)

    nqb = ML_WIDTH // ML_QKV_BLOCK
    ml_w_in = nrm((N_LAYERS_C, D, 2 * ML_WIDTH), D ** -0.5)
    ml_conv_w = nrm((N_LAYERS_C, CONV_W, ML_WIDTH), CONV_W ** -0.5)
    ml_conv_b = nrm((N_LAYERS_C, ML_WIDTH), 0.02)
    ml_w_q = nrm((N_LAYERS_C, nqb, ML_QKV_BLOCK, ML_QKV_BLOCK), ML_QKV_BLOCK ** -0.5)
    ml_w_k = nrm((N_LAYERS_C, nqb, ML_QKV_BLOCK, ML_QKV_BLOCK), ML_QKV_BLOCK ** -0.5)
    ml_w_v = nrm((N_LAYERS_C, nqb, ML_QKV_BLOCK, ML_QKV_BLOCK), ML_QKV_BLOCK ** -0.5)
    ml_w_gates = nrm((N_LAYERS_C, 3 * ML_WIDTH, 2 * H), (3 * ML_WIDTH) ** -0.5)
    f_bias = jnp.broadcast_to(jnp.linspace(3.0, 6.0, H, dtype=F32), (N_LAYERS_C, H))
    ml_b_gates = jnp.concatenate([nrm((N_LAYERS_C, H), 0.1), f_bias + nrm((N_LAYERS_C, H), 0.02)], axis=-1)
    ml_skip = 1.0 + nrm((N_LAYERS_C, ML_WIDTH), 0.02)
    ml_norm_g = 1.0 + nrm((N_LAYERS_C, ML_WIDTH), 0.02)
    ml_w_out = nrm((N_LAYERS_C, ML_WIDTH, D), DN_BETA * ML_WIDTH ** -0.5)

    moe_w_router = nrm((DEPTH, D, N_EXPERTS), D ** -0.5)
    moe_b_router = nrm((DEPTH, N_EXPERTS), 0.01)
    moe_w_gate_up = nrm((DEPTH, N_EXPERTS, D, 2 * D_EXPERT), D ** -0.5)
    moe_b_gate_up = nrm((DEPTH, N_EXPERTS, 2 * D_EXPERT), 0.02)
    moe_w_down = nrm((DEPTH, N_EXPERTS, D_EXPERT, D), DN_BETA * D_EXPERT ** -0.5)
    moe_b_down = nrm((DEPTH, N_EXPERTS, D), 0.02)

    return {
        "x": x, "c": c, "ada_w": ada_w, "ada_b": ada_b,
        "ln1_g": ln1_g, "ln1_b": ln1_b, "ln2_g": ln2_g, "ln2_b": ln2_b,
        "rg_w_in": rg_w_in, "rg_conv_w": rg_conv_w, "rg_conv_b": rg_conv_b,
        "rg_w_rgate": rg_w_rgate, "rg_b_rgate": rg_b_rgate, "rg_w_igate": rg_w_igate,
        "rg_b_igate": rg_b_igate, "rg_lam": rg_lam, "rg_w_out": rg_w_out,
        "sg_w_in": sg_w_in, "sg_b_in": sg_b_in, "sg_ln_g": sg_ln_g, "sg_ln_b": sg_ln_b,
        "sg_w_sp": sg_w_sp, "sg_b_sp": sg_b_sp, "sg_w_out": sg_w_out, "sg_b_out": sg_b_out,
        "ml_w_in": ml_w_in, "ml_conv_w": ml_conv_w, "ml_conv_b": ml_conv_b,
        "ml_w_q": ml_w_q, "ml_w_k": ml_w_k, "ml_w_v": ml_w_v, "ml_w_gates": ml_w_gates,
        "ml_b_gates": ml_b_gates, "ml_skip": ml_skip, "ml_norm_g": ml_norm_g, "ml_w_out": ml_w_out,
        "moe_w_router": moe_w_router, "moe_b_router": moe_b_router,
        "moe_w_gate_up": moe_w_gate_up, "moe_b_gate_up": moe_b_gate_up,
        "moe_w_down": moe_w_down, "moe_b_down": moe_b_down,
    }


def reference(x, c, ada_w, ada_b, ln1_g, ln1_b, ln2_g, ln2_b,
              rg_w_in, rg_conv_w, rg_conv_b, rg_w_rgate, rg_b_rgate, rg_w_igate, rg_b_igate, rg_lam, rg_w_out,
              sg_w_in, sg_b_in, sg_ln_g, sg_ln_b, sg_w_sp, sg_b_sp, sg_w_out, sg_b_out,
              ml_w_in, ml_conv_w, ml_conv_b, ml_w_q, ml_w_k, ml_w_v, ml_w_gates, ml_b_gates, ml_skip,
              ml_norm_g, ml_w_out,
              moe_w_router, moe_b_router, moe_w_gate_up, moe_b_gate_up, moe_w_down, moe_b_down):
    cond = jax.nn.silu(c)
    for i in range(DEPTH):
        mod = (cond @ ada_w[i] + ada_b[i])[:, None, :]
        sh1, sc1, g1, sh2, sc2, g2 = jnp.split(mod, 6, axis=-1)
        h = x * (1 + sc1) + sh1
        kind, j = i % N_MIXERS, i // N_MIXERS
        if kind == 0:
            y = rglru_mixer(h, rg_w_in[j], rg_conv_w[j], rg_conv_b[j], rg_w_rgate[j], rg_b_rgate[j],
                            rg_w_igate[j], rg_b_igate[j], rg_lam[j], rg_w_out[j])
        elif kind == 1:
            y = sgu_mixer(h, sg_w_in[j], sg_b_in[j], sg_ln_g[j], sg_ln_b[j], sg_w_sp[j], sg_b_sp[j],
                          sg_w_out[j], sg_b_out[j])
        else:
            y = mlstm_mixer(h, ml_w_in[j], ml_conv_w[j], ml_conv_b[j], ml_w_q[j], ml_w_k[j], ml_w_v[j],
                            ml_w_gates[j], ml_b_gates[j], ml_skip[j], ml_norm_g[j], ml_w_out[j])
        x = layer_norm(DN_ALPHA * x + (1 + g1) * y, ln1_g[i], ln1_b[i])
        h = x * (1 + sc2) + sh2
        y = moe_ffn(h, moe_w_router[i], moe_b_router[i], moe_w_gate_up[i], moe_b_gate_up[i],
                    moe_w_down[i], moe_b_down[i])
        x = layer_norm(DN_ALPHA * x + (1 + g2) * y, ln2_g[i], ln2_b[i])
    return x
```

```python
import numpy as np
from contextlib import ExitStack
import concourse.bass as bass
import concourse.mybir as mybir
from concourse.bass_utils import run_bass_kernel_spmd

F32 = mybir.dt.float32
BF16 = mybir.dt.bfloat16
AF = mybir.ActivationFunctionType
ALU = mybir.AluOpType

D = 2048
KT = 16
CT = 512
RGW = 2560
RGT = 20
NE = 32
DE = 512
ALPHA = 8 ** 0.25
LN_EPS = 1e-5
SAME_ENG_SYNC = True


def dsize(dt):
    return 2 if dt == BF16 else 4


class View:
    __slots__ = ("ap", "keys", "off")

    def __init__(self, ap, keys, off=None):
        self.ap = ap
        self.keys = keys
        self.off = off

    def __getitem__(self, idx):
        return View(self.ap[idx], self.keys)

    def re(self, pat, **kw):
        return View(self.ap.rearrange(pat, **kw), self.keys)

    def bc(self, shape):
        return View(self.ap.broadcast_to(shape), self.keys)


class Prog:
    ENGS = ("pe", "act", "dve", "pool", "sp")
    NDMA = {"sp": 12, "pool": 12}

    def __init__(self, nc, arena_kib=168):
        self.nc = nc
        self.es = ExitStack()
        self.streams = {e: [] for e in self.ENGS}
        self.cnt = {e: 0 for e in self.ENGS}
        self.clock = {e: {} for e in self.ENGS}
        self.dcnt = {}
        self.drr = {q: 0 for q in self.NDMA}
        self.lw = {}
        self.rd = {}
        self.arena_bytes = arena_kib * 1024
        self.arena = self.es.enter_context(nc.sbuf_tensor("arena", [128, self.arena_bytes // 4], F32))
        self.top = 0
        self.psb = [self.es.enter_context(nc.psum_tensor(f"psb{i}", [128, 512], F32)) for i in range(8)]
        self.ninst = 0

    def alloc(self, shape, dt, at=None):
        n = int(np.prod(shape)) * dsize(dt)
        nr = (n + 1023) // 1024 * 1024
        if at is None:
            off = self.top
            self.top += nr
            assert self.top <= self.arena_bytes, f"arena overflow {self.top}"
        else:
            off = at
            assert off % 1024 == 0 and off + nr <= self.arena_bytes
        ap = self.arena[:, off // 4:(off + n + 3) // 4]
        if dt != F32:
            ap = ap.bitcast(dt)
        if len(shape) == 2:
            ap = ap.rearrange("p (a b) -> p a b", a=shape[0])
        elif len(shape) == 3:
            ap = ap.rearrange("p (a b c) -> p a b c", a=shape[0], b=shape[1])
        return View(ap, [("a", p) for p in range(off // 1024, (off + nr) // 1024)], off)

    def mark(self):
        return self.top

    def release(self, m):
        self.top = m

    def sb(self, name, shape, dt):
        t = self.es.enter_context(self.nc.sbuf_tensor(name, list(shape), dt))
        return View(t[:], [("t", name)])

    def ps(self, b, dt=F32):
        return View(self.psb[b][:], [("ps", b)])

    def dram(self, name, shape, dt, kind="Internal"):
        t = self.nc.dram_tensor(name, list(shape), dt, kind=kind).ap()
        return t

    def _deps(self, reads, writes):
        deps = []
        for v in reads:
            for k in v.keys:
                e = self.lw.get(k)
                if e is not None:
                    deps.append(e)
        for v in writes:
            for k in v.keys:
                e = self.lw.get(k)
                if e is not None:
                    deps.append(e)
                r = self.rd.get(k)
                if r:
                    deps.extend(r)
        return deps

    def _waits(self, eng, deps):
        clk = self.clock[eng]
        need = {}
        for (sem, val, peng) in deps:
            if peng == eng and (eng == "pe" or not SAME_ENG_SYNC):
                continue
            if clk.get(sem, 0) >= val:
                continue
            if need.get(sem, 0) < val:
                need[sem] = val
        for sem, val in need.items():
            clk[sem] = val
        return list(need.items())

    def _record(self, ev, reads, writes):
        for v in writes:
            for k in v.keys:
                self.lw[k] = ev
                self.rd[k] = []
        for v in reads:
            for k in v.keys:
                lst = self.rd.setdefault(k, [])
                lst[:] = [x for x in lst if x[0] != ev[0]]
                lst.append(ev)

    def op(self, eng, fn, reads, writes):
        reads = [v for v in reads if isinstance(v, View)]
        waits = self._waits(eng, self._deps(reads, writes))
        self.cnt[eng] += 1
        ev = (eng, self.cnt[eng], eng)
        self.streams[eng].append((waits, fn, (eng, 1)))
        self._record(ev, reads, writes)
        self.ninst += 1
        return ev

    def dma(self, q, out, in_):
        i = self.drr[q]
        self.drr[q] = (i + 1) % self.NDMA[q]
        sem = f"d{q}{i}"
        c = self.dcnt.get(sem, 0)
        deps = self._deps([in_], [out])
        if c > 0:
            deps.append((sem, 16 * c, "dma"))
        waits = self._waits(q, deps)
        self.dcnt[sem] = c + 1
        ev = (sem, 16 * (c + 1), "dma")
        oap, iap = out.ap, in_.ap
        self.streams[q].append((waits, lambda e: e.dma_start(out=oap, in_=iap), (sem, 16)))
        self._record(ev, [in_], [out])
        self.ninst += 1
        return ev

    def wait_all(self, eng, evs):
        waits = self._waits(eng, evs)
        self.streams[eng].append((waits, None, None))

    def emit(self):
        nc = self.nc
        names = set(self.ENGS) | set(self.dcnt.keys())
        sems = {n: self.es.enter_context(nc.semaphore(n)) for n in sorted(names)}
        block = self.es.enter_context(nc.Block())

        def runner(en):
            def f(eng):
                for waits, fn, inc in self.streams[en]:
                    if fn is None:
                        for (s, v) in waits:
                            eng.wait_ge(sems[s], v)
                        continue
                    for (s, v) in waits[1:]:
                        eng.wait_ge(sems[s], v)
                    ins = fn(eng)
                    if waits:
                        ins._wait_ge(sems[waits[0][0]], waits[0][1])
                    ins.then_inc(sems[inc[0]], inc[1])
            return f
        block.tensor(runner("pe"))
        block.scalar(runner("act"))
        block.vector(runner("dve"))
        block.gpsimd(runner("pool"))
        block.sync(runner("sp"))
        self.es.close()

    @staticmethod
    def _a(x):
        return x.ap if isinstance(x, View) else x

    def mm(self, out, lhsT, rhs, start=True, stop=True):
        o, l, r = out.ap, lhsT.ap, rhs.ap
        return self.op("pe", lambda e: e.matmul(o, lhsT=l, rhs=r, start=start, stop=stop), [lhsT, rhs], [out])

    def tr(self, out, in_, ident):
        o, i, d = out.ap, in_.ap, ident.ap
        return self.op("pe", lambda e: e.transpose(o, i, d), [in_, ident], [out])

    def act(self, out, in_, func, bias=0.0, scale=1.0, accum=None):
        o, i, b, s = out.ap, in_.ap, self._a(bias), self._a(scale)
        if accum is not None:
            a = accum.ap
            return self.op("act", lambda e: e.activation(out=o, in_=i, func=func, bias=b, scale=s, accum_out=a), [in_, bias, scale], [out, accum])
        return self.op("act", lambda e: e.activation(out=o, in_=i, func=func, bias=b, scale=s), [in_, bias, scale], [out])

    def reduce_add(self, out, in_):
        o, i = out.ap, in_.ap
        return self.op("dve", lambda e: e.tensor_reduce(out=o, in_=i, axis=mybir.AxisListType.X, op=ALU.add), [in_], [out])

    def tt(self, eng, out, in0, in1, op):
        o, a, b = out.ap, in0.ap, in1.ap
        return self.op(eng, lambda e: e.tensor_tensor(out=o, in0=a, in1=b, op=op), [in0, in1], [out])

    def ts(self, eng, out, in0, s1, op0, s2=None, op1=None):
        o, a, x1, x2 = out.ap, in0.ap, self._a(s1), self._a(s2)
        if op1 is None:
            return self.op(eng, lambda e: e.tensor_scalar(out=o, in0=a, scalar1=x1, scalar2=None, op0=op0), [in0, s1], [out])
        return self.op(eng, lambda e: e.tensor_scalar(out=o, in0=a, scalar1=x1, scalar2=x2, op0=op0, op1=op1), [in0, s1, s2], [out])

    def stt(self, eng, out, in0, scalar, in1, op0, op1):
        o, a, s, b = out.ap, in0.ap, self._a(scalar), in1.ap
        return self.op(eng, lambda e: e.scalar_tensor_tensor(out=o, in0=a, scalar=s, in1=b, op0=op0, op1=op1), [in0, scalar, in1], [out])

    def copy(self, eng, out, in_):
        o, i = out.ap, in_.ap
        if eng == "act":
            return self.op("act", lambda e: e.copy(out=o, in_=i), [in_], [out])
        return self.op(eng, lambda e: e.tensor_copy(out=o, in_=i), [in_], [out])

    def memset(self, eng, out, val):
        o = out.ap
        return self.op(eng, lambda e: e.memset(o, val), [], [out])

    def scan(self, out, d0, d1, init, op0, op1):
        o, a, b, i = out.ap, d0.ap, d1.ap, self._a(init)
        return self.op("dve", lambda e: e.tensor_tensor_scan(out=o, data0=a, data1=b, initial=i, op0=op0, op1=op1), [d0, d1, init], [out])

    def recip(self, out, in_):
        o, i = out.ap, in_.ap
        return self.op("dve", lambda e: e.reciprocal(out=o, in_=i), [in_], [out])

    def max8(self, out, in_):
        o, i = out.ap, in_.ap
        return self.op("dve", lambda e: e.max(out=o, in_=i), [in_], [out])

    def bn_stats(self, out, in_):
        o, i = out.ap, in_.ap
        return self.op("dve", lambda e: e.bn_stats(out=o, in_=i), [in_], [out])

    def bn_aggr(self, out, in_):
        o, i = out.ap, in_.ap
        return self.op("dve", lambda e: e.bn_aggr(out=o, in_=i), [in_], [out])


def DV(ap, name, region=0):
    return View(ap, [("d", name, region)])


class MK:
    def __init__(self, NSEQ=2, S=2048, layers=None, dbg=None):
        self.NSEQ, self.S = NSEQ, S
        self.NTOK = NSEQ * S
        self.NCT = self.NTOK // CT
        self.CPS = S // CT
        self.layers = layers if layers is not None else [(0, 0), (1, 0), (2, 0), (0, 1)]
        self.L = len(self.layers)
        self.nA = sum(1 for k, _ in self.layers if k == 0)
        self.nB = sum(1 for k, _ in self.layers if k == 1)
        self.nC = sum(1 for k, _ in self.layers if k == 2)
        self.dbg = dbg
        nc = self.nc = bass.Bass("TRN2", target_bir_lowering=False)
        self.P = Prog(nc, arena_kib=176)
        self.inputs = {}
        self.build()

    def inp(self, name, shape, dt=F32):
        ap = self.nc.dram_tensor(name, list(shape), dt, kind="ExternalInput").ap()
        self.inputs[name] = (tuple(shape), ap)
        return ap

    def build(self):
        P, L, NT = self.P, self.L, self.NTOK
        nA, nB, nC = max(self.nA, 1), max(self.nB, 1), max(self.nC, 1)
        self.xT = self.inp("xT", [D, NT])
        self.cT = self.inp("cT", [128, KT, self.NSEQ])
        self.ada_w = self.inp("ada_w", [L, D, 6 * D])
        self.ada_bT = self.inp("ada_bT", [128, L, 96])
        self.lnp = self.inp("lnp", [128, L, 4, KT])
        self.ident_d = self.inp("ident", [128, 128])
        self.maskneg_d = self.inp("maskneg", [128, 4, 512])
        self.rg_w_in = self.inp("rg_w_in", [nA, D, 2 * RGW])
        self.rg_vec = self.inp("rg_vec", [128, nA, 9, RGT])
        self.rg_slabs = self.inp("rg_slabs", [nA, RGT, 128, 2, 4, 128])
        self.rg_w_out = self.inp("rg_w_out", [nA, RGW, D])
        self.sg_w_in = self.inp("sg_w_in", [nB, D, 2 * D])
        self.sg_vecp = self.inp("sg_vecp", [128, nB, 2, KT])
        self.sg_rows = self.inp("sg_rows", [nB, 4, D])
        self.sg_w_spT = self.inp("sg_w_spT", [nB, 128, 8, 128])
        self.sg_w_out = self.inp("sg_w_out", [nB, D, D])
        self.ml_w_in = self.inp("ml_w_in", [nC, D, 2 * D])
        self.ml_vecp = self.inp("ml_vecp", [128, nC, 7, KT])
        self.ml_bd = self.inp("ml_bd", [nC, 3, 128, KT, 128])
        self.ml_w_gates = self.inp("ml_w_gates", [nC, 128, 48, 16])
        self.ml_b_gates = self.inp("ml_b_gates", [8, nC, 2])
        self.ml_w_out = self.inp("ml_w_out", [nC, D, D])
        self.moe_w_router = self.inp("moe_w_router", [L, 128, KT, NE])
        self.moe_b_router = self.inp("moe_b_router", [L, NE, 1])
        self.moe_w_gate_up = self.inp("moe_w_gate_up", [L, NE, D, 2 * DE])
        self.moe_b_gu = self.inp("moe_b_gu", [128, L, NE, 8])
        self.moe_w_down = self.inp("moe_w_down", [L, NE, DE, D])
        self.moe_b_down = self.inp("moe_b_down", [L, NE, D])
        self.outT = self.nc.dram_tensor("outT", [D, NT], F32, kind="ExternalOutput").ap()
        self.X1 = P.dram("X1", [D, NT], F32)
        self.X2 = P.dram("X2", [D, NT], F32)
        self.YT = P.dram("YT", [D, NT], F32)

        self.setup_consts()
        self.compute_mods()
        xin = self.xT
        xin_name = "xT"
        for li, (kind, j) in enumerate(self.layers):
            last = li == self.L - 1
            if kind == 0:
                self.rg_layer(li, j, xin, xin_name)
            elif kind == 1:
                self.sgu_layer(li, j, xin, xin_name)
            else:
                self.ml_layer(li, j, xin, xin_name)
            dst, dname = (self.outT, "outT") if last else (self.X2, "X2")
            self.moe_layer(li, dst, dname)
            xin, xin_name = self.X2, "X2"
        evs = []
        for k, e in P.lw.items():
            if k[0] == "d" and k[1] == "outT":
                evs.append(e)
        P.wait_all("sp", evs)
        P.emit()

    def dv(self, ap, name, ct):
        return DV(ap, name, ct)

    def setup_consts(self):
        P = self.P
        self.ident = P.sb("identc", [128, 128], F32)
        P.dma("sp", self.ident, DV(self.ident_d, "ident"))
        self.onesD = P.sb("onesD", [128, 128], F32)
        P.memset("pool", self.onesD, 1.0 / D)
        self.onec = P.sb("onec", [128, 1], F32)
        P.memset("pool", self.onec, 1.0)
        self.epsc = P.sb("epsc", [128, 2], F32)
        P.memset("pool", self.epsc[:, 0:1], LN_EPS / (ALPHA * ALPHA))
        P.memset("pool", self.epsc[:, 1:2], LN_EPS)
        self.MOD = P.sb("MOD", [128, self.L, 6, KT, self.NSEQ], F32)
        self.LNP = P.sb("LNP", [128, self.L, 4, KT], F32)
        P.dma("sp", self.LNP, DV(self.lnp, "lnp"))
        self.SEL = P.sb("SEL", [32, 2, 128], F32)
        self.lnmean = P.sb("lnmean", [128, 512], F32)
        self.lnrstd = P.sb("lnrstd", [128, 512], F32)
        self.lntmp = P.sb("lntmp", [128, 2, 512], F32)

    def compute_mods(self):
        P, L, NS = self.P, self.L, self.NSEQ
        m = P.mark()
        cT = P.alloc([KT, NS], F32)
        P.dma("sp", cT, DV(self.cT, "cT"))
        cond = P.alloc([KT, NS], F32)
        P.act(cond, cT, AF.Silu)
        abT = P.alloc([L, 96], F32)
        P.dma("sp", abT, DV(self.ada_bT, "ada_bT"))
        wb = [P.alloc([D], F32) for _ in range(3)]
        acc = P.alloc([KT, NS], F32)
        n = 0
        for l in range(L):
            for c6 in range(6):
                for k in range(KT):
                    w = wb[n % 3]
                    n += 1
                    P.dma("sp", w, DV(self.ada_w[l, k * 128:(k + 1) * 128, c6 * D:(c6 + 1) * D], "ada_w"))
                    ps = P.ps(k % 2)
                    for f in range(KT):
                        P.mm(ps[:, f * NS:(f + 1) * NS], w[:, f * 128:(f + 1) * 128], cond[:, k, :])
                    pv = ps[:, 0:KT * NS].re("p (f s) -> p f s", s=NS)
                    if k == 0:
                        P.copy("dve", acc, pv)
                    else:
                        P.tt("dve", acc, acc, pv, ALU.add)
                bias = abT[:, l, c6 * KT:(c6 + 1) * KT]
                bb = View(bias.ap.unsqueeze(2).broadcast_to([128, KT, NS]), bias.keys)
                dst = self.MOD[:, l, c6]
                if c6 in (1, 4):
                    P.stt("dve", dst, acc, 1.0, bb, ALU.add, ALU.add)
                elif c6 in (2, 5):
                    P.tt("dve", dst, acc, bb, ALU.add)
                    P.ts("dve", dst, dst, 1.0, ALU.add, 1.0 / ALPHA, ALU.mult)
                else:
                    P.tt("dve", dst, acc, bb, ALU.add)
        P.release(m)

    def mod(self, l, c6, k, s):
        return self.MOD[:, l, c6, k, s:s + 1]

    def resid_ln(self, li, which, ct, V, dst, dname):
        P = self.P
        psm, pse = P.ps(6), P.ps(7)
        for k in range(KT):
            P.mm(psm, self.onesD, V[k], start=(k == 0), stop=(k == KT - 1))
        for k in range(KT):
            sq = self.lntmp[:, k % 2]
            P.act(sq, V[k], AF.Square)
            P.mm(pse, self.onesD, sq, start=(k == 0), stop=(k == KT - 1))
        mean, rstd = self.lnmean, self.lnrstd
        P.copy("act", mean, psm)
        m2 = self.lntmp[:, 0]
        P.tt("pool", m2, mean, mean, ALU.mult)
        P.tt("dve", rstd, pse, m2, ALU.subtract)
        P.act(rstd, rstd, AF.Sqrt, bias=self.epsc[:, 0:1])
        P.recip(rstd, rstd)
        gi = 0 if which == 1 else 2
        for k in range(KT):
            P.tt("dve", V[k], V[k], mean, ALU.subtract)
            P.tt("pool", V[k], V[k], rstd, ALU.mult)
            P.act(V[k], V[k], AF.Identity, bias=self.LNP[:, li, gi + 1, k:k + 1], scale=self.LNP[:, li, gi, k:k + 1])
            P.dma("sp", self.dv(dst[k * 128:(k + 1) * 128, ct * CT:(ct + 1) * CT], dname, ct), V[k])

    def load_x_tiles(self, src, sname, ct, X):
        for k in range(KT):
            self.P.dma("sp", X[k], self.dv(src[k * 128:(k + 1) * 128, ct * CT:(ct + 1) * CT], sname, ct))

    def modulate(self, li, which, ct, X, H):
        s = ct // self.CPS
        c0 = 0 if which == 1 else 3
        for k in range(KT):
            self.P.act(H[k], X[k], AF.Identity, bias=self.mod(li, c0, k, s), scale=self.mod(li, c0 + 1, k, s))

    def wload(self, buf, src_ap, kt, name):
        step = 8
        for k0 in range(0, kt, step):
            k1 = min(kt, k0 + step)
            self.P.dma("pool", buf[:, k0:k1], DV(src_ap[k0 * 128:k1 * 128, :].rearrange("(k p) n -> p k n", p=128), name))

    def moe_layer(self, li, dst, dname):
        P = self.P
        TG = 2 * CT
        for g in range(self.NTOK // TG):
            m0 = P.mark()
            cts = (2 * g, 2 * g + 1)
            H2 = [P.alloc([TG], BF16) for _ in range(KT)]
            ACC = [[P.alloc([CT], F32) for _ in range(2)] for _ in range(KT)]
            CBT = P.alloc([TG], F32)
            BGU = P.alloc([NE, 8], F32)
            P.dma("sp", BGU, DV(self.moe_b_gu[:, li], "moe_b_gu"))
            m1 = P.mark()
            WR = P.alloc([KT, NE], F32)
            P.dma("sp", WR, DV(self.moe_w_router[li], "moe_w_router"))
            BR = P.alloc([1], F32)
            P.dma("sp", BR[0:32], DV(self.moe_b_router[li], "moe_b_router"))
            BD = P.alloc([D], F32)
            P.dma("sp", BD[0:32], DV(self.moe_b_down[li], "moe_b_down"))
            XS = [P.alloc([TG], F32) for _ in range(2)]
            HF = [P.alloc([TG], F32) for _ in range(2)]
            for k in range(KT):
                xs, hf = XS[k % 2], HF[k % 2]
                for c in range(2):
                    ct = cts[c]
                    s = ct // self.CPS
                    P.dma("sp", xs[:, c * CT:(c + 1) * CT], self.dv(self.X1[k * 128:(k + 1) * 128, ct * CT:(ct + 1) * CT], "X1", ct))
                    P.act(hf[:, c * CT:(c + 1) * CT], xs[:, c * CT:(c + 1) * CT], AF.Identity,
                          bias=self.mod(li, 3, k, s), scale=self.mod(li, 4, k, s))
                P.copy("dve", H2[k], hf)
                for c in range(2):
                    P.mm(P.ps(c)[0:32], WR[:, k, :], hf[:, c * CT:(c + 1) * CT], start=(k == 0), stop=(k == KT - 1))
            LT = P.alloc([TG], F32)
            for c in range(2):
                P.act(LT[0:32, c * CT:(c + 1) * CT], P.ps(c)[0:32], AF.Identity, bias=BR[0:32, 0:1])
            NTT = TG // 128
            LG = P.alloc([NTT, NE], F32)
            pt = P.ps(2)
            for tt in range(NTT):
                P.tr(pt[:, tt * NE:(tt + 1) * NE], LT[0:32, tt * 128:(tt + 1) * 128], self.ident[0:32, 0:32])
            P.copy("dve", LG, pt[:, 0:NTT * NE].re("p (t e) -> p t e", e=NE))
            M8 = P.alloc([NTT, 8], F32)
            for tt in range(NTT):
                P.max8(M8[:, tt, :], LG[:, tt, :])
            MSK = P.alloc([NTT, NE], F32)
            EX = P.alloc([NTT, NE], F32)
            NM = P.alloc([NTT], F32)
            P.ts("dve", NM, M8[:, :, 0], -1.0, ALU.mult)
            for tt in range(NTT):
                P.ts("dve", MSK[:, tt, :], LG[:, tt, :], M8[:, tt, 3:4], ALU.is_ge)
                P.act(EX[:, tt, :], LG[:, tt, :], AF.Exp, bias=NM[:, tt:tt + 1])
            P.tt("dve", EX, EX, MSK, ALU.mult)
            SM = P.alloc([NTT], F32)
            sm_o, ex_i = SM.ap, EX.ap
            P.op("dve", lambda e: e.tensor_reduce(out=sm_o, in_=ex_i, axis=mybir.AxisListType.X, op=ALU.add), [EX], [SM])
            P.recip(SM, SM)
            P.tt("dve", EX, EX, View(SM.ap.unsqueeze(2).broadcast_to([128, NTT, NE]), SM.keys), ALU.mult)
            for tt in range(NTT):
                c = tt // 4
                P.tr(P.ps(c)[0:32, (tt % 4) * 128:(tt % 4 + 1) * 128], EX[:, tt, :], self.ident)
            for c in range(2):
                P.copy("act", CBT[0:32, c * CT:(c + 1) * CT], P.ps(c)[0:32])
            n = 0
            for f in range(KT):
                for c in range(2):
                    ps = P.ps(4 + n % 2)
                    n += 1
                    P.mm(ps, BD[0:32, f * 128:(f + 1) * 128], CBT[0:32, c * CT:(c + 1) * CT])
                    P.copy("act", ACC[f][c], ps)
            P.release(m1)
            WGU = [P.alloc([KT, 2, 256], BF16) for _ in range(2)]
            WDN = P.alloc([4, D], BF16)
            A = [[P.alloc([CT], BF16) for _ in range(4)] for _ in range(2)]
            CBE = [P.alloc([CT], F32) for _ in range(2)]
            Gb = [P.alloc([CT], F32) for _ in range(2)]
            Ub = [P.alloc([CT], F32) for _ in range(2)]
            Sb = [P.alloc([CT], F32) for _ in range(2)]
            nw = 0
            nm = 0
            nd = 0
            for e in range(NE):
                wsrc = self.moe_w_gate_up[li, e]
                sel = self.SEL[:, e % 2, :]
                P.copy("pool", sel, View(self.ident.ap[0:32, e:e + 1].broadcast_to([32, 128]), self.ident.keys))
                for c in range(2):
                    ps = P.ps(6)
                    P.mm(ps, sel, CBT[0:32, c * CT:(c + 1) * CT])
                    P.copy("act", CBE[c], ps)
                for mp in range(2):
                    w = WGU[nw % 2]
                    nw += 1
                    for gu in range(2):
                        col0 = gu * DE + mp * 256
                        for k0 in (0, 8):
                            P.dma("pool", w[:, k0:k0 + 8, gu, :],
                                  DV(wsrc[k0 * 128:(k0 + 8) * 128, col0:col0 + 256].rearrange("(k p) n -> p k n", p=128), "moe_w_gate_up"))
                    for c in range(2):
                        for mi in range(2):
                            mt = mp * 2 + mi
                            psG, psU = P.ps(nm % 2), P.ps(2 + nm % 2)
                            G, U, S_ = Gb[nm % 2], Ub[nm % 2], Sb[nm % 2]
                            nm += 1
                            for k in range(KT):
                                P.mm(psG, w[:, k, 0, mi * 128:(mi + 1) * 128], H2[k][:, c * CT:(c + 1) * CT], start=(k == 0), stop=(k == KT - 1))
                            for k in range(KT):
                                P.mm(psU, w[:, k, 1, mi * 128:(mi + 1) * 128], H2[k][:, c * CT:(c + 1) * CT], start=(k == 0), stop=(k == KT - 1))
                            P.ts("dve", G, psG, BGU[:, e, mt:mt + 1], ALU.add, 7.0, ALU.min)
                            P.act(S_, G, AF.Sigmoid, scale=1.702)
                            P.ts("dve", U, psU, BGU[:, e, 4 + mt:5 + mt], ALU.add, 7.0, ALU.min)
                            P.ts("pool", U, U, -7.0, ALU.max, 1.0, ALU.add)
                            P.tt("pool", G, G, S_, ALU.mult)
                            P.tt("pool", U, U, CBE[c], ALU.mult)
                            P.tt("dve", A[c][mt], G, U, ALU.mult)
                self.wload(WDN, self.moe_w_down[li, e], 4, "moe_w_down")
                for c in range(2):
                    for f in range(KT):
                        ps = P.ps(4 + nd % 2)
                        nd += 1
                        for k in range(4):
                            P.mm(ps, WDN[:, k, f * 128:(f + 1) * 128], A[c][k], start=(k == 0), stop=(k == 3))
                        P.tt("dve", ACC[f][c], ACC[f][c], ps, ALU.add)
            for c in range(2):
                ct = cts[c]
                s = ct // self.CPS
                V = []
                for f in range(KT):
                    xs = XS[f % 2][:, c * CT:(c + 1) * CT] if False else None
                    xt = CBE[0] if False else None
                    V.append(ACC[f][c])
                xbuf = [Gb[0], Gb[1], Ub[0], Ub[1]]
                for f in range(KT):
                    xb = xbuf[f % 4]
                    P.dma("sp", xb, self.dv(self.X1[f * 128:(f + 1) * 128, ct * CT:(ct + 1) * CT], "X1", ct))
                    P.stt("dve", V[f], V[f], self.mod(li, 5, f, s), xb, ALU.mult, ALU.add)
                self.resid_ln(li, 2, ct, V, dst, dname)
            P.release(m0)

    def rg_slab_tiles(self, j):
        b0, b1 = (128 * j) // 160, (128 * j + 127) // 160
        i0, i1 = (160 * b0) // 128, (160 * b1 + 159) // 128
        return list(range(i0, i1 + 1))

    def rg_layer(self, li, j, xsrc, xname):
        P = self.P
        m0 = P.mark()
        VEC = P.sb(f"rgvec{li}", [128, 9, RGT], F32)
        P.dma("sp", VEC, DV(self.rg_vec[:, j], "rg_vec"))
        cfe = VEC[:, 8, :]
        P.act(cfe, VEC[:, 7, :], AF.Exp, scale=-1.0)
        P.act(cfe, cfe, AF.Ln, bias=1.0)
        P.ts("dve", cfe, cfe, -8.0, ALU.mult)
        HST = P.sb(f"rghst{li}", [128, RGT], F32)
        HALO = P.sb(f"rghalo{li}", [128, RGT, 3], F32)
        X = [P.alloc([CT], F32) for _ in range(KT)]
        H = [P.alloc([CT], BF16) for _ in range(KT)]
        WB = [P.alloc([RGT, CT], BF16) for _ in range(2)]
        GATE = [P.alloc([CT], BF16) for _ in range(RGT)]
        XRB = [P.alloc([CT], BF16) for _ in range(RGT)]
        XRF = [P.alloc([CT], F32) for _ in range(2)]
        RECB = [P.alloc([CT + 4], F32) for _ in range(2)]
        SL = [P.alloc([2, 4, 128], BF16) for _ in range(2)]
        Rb = [P.alloc([CT], F32) for _ in range(2)]
        Ib = [P.alloc([CT], F32) for _ in range(2)]
        Ab = [P.alloc([CT], F32) for _ in range(2)]
        Tb = [P.alloc([CT], F32) for _ in range(2)]
        HS = [P.alloc([CT], F32) for _ in range(2)]
        nw = 0
        npz = 0
        for ct in range(self.NCT):
            s = ct // self.CPS
            if ct % self.CPS == 0:
                P.memset("pool", HST, 0.0)
                P.memset("pool", HALO, 0.0)
            self.load_x_tiles(xsrc, xname, ct, X)
            self.modulate(li, 1, ct, X, H)
            for nb in range(10):
                w = WB[nw % 2]
                nw += 1
                self.wload(w[:, 0:KT], self.rg_w_in[j][:, nb * CT:(nb + 1) * CT], KT, "rg_w_in")
                for mi in range(4):
                    ft = nb * 4 + mi
                    ps = P.ps(npz % 4)
                    npz += 1
                    for k in range(KT):
                        P.mm(ps, w[:, k, mi * 128:(mi + 1) * 128], H[k], start=(k == 0), stop=(k == KT - 1))
                    if ft < RGT:
                        P.act(GATE[ft], ps, AF.Gelu_apprx_tanh)
                    else:
                        jj = ft - RGT
                        rb = RECB[jj % 2]
                        xr = XRF[jj % 2]
                        P.copy("act", rb[:, 3:3 + CT], ps)
                        P.copy("pool", rb[:, 0:3], HALO[:, jj, :])
                        P.copy("pool", HALO[:, jj, :], rb[:, CT:CT + 3])
                        P.act(xr, rb[:, 3:3 + CT], AF.Identity, bias=VEC[:, 4, jj:jj + 1], scale=VEC[:, 3, jj:jj + 1])
                        for d in (1, 2, 3):
                            P.stt("dve", xr, rb[:, 3 - d:3 - d + CT], VEC[:, 3 - d, jj:jj + 1], xr, ALU.mult, ALU.add)
                        P.copy("act", XRB[jj], xr)
            for jt in range(RGT):
                tiles = self.rg_slab_tiles(jt)
                sl = SL[jt % 2]
                P.dma("pool", sl, DV(self.rg_slabs[j, jt], "rg_slabs"))
                psR, psI = P.ps(4 + 2 * (jt % 2)), P.ps(5 + 2 * (jt % 2))
                for gi, ps in ((0, psR), (1, psI)):
                    for si, it in enumerate(tiles):
                        P.mm(ps, sl[:, gi, si, :], XRB[it], start=(si == 0), stop=(si == len(tiles) - 1))
                R, I, A_, T, hs = Rb[jt % 2], Ib[jt % 2], Ab[jt % 2], Tb[jt % 2], HS[jt % 2]
                P.act(R, psR, AF.Sigmoid, bias=VEC[:, 5, jt:jt + 1])
                P.act(I, psI, AF.Sigmoid, bias=VEC[:, 6, jt:jt + 1])
                P.act(A_, R, AF.Exp, scale=VEC[:, 8, jt:jt + 1])
                P.tt("pool", T, A_, A_, ALU.mult)
                P.ts("pool", T, T, -1.0, ALU.mult, 1.0, ALU.add)
                P.act(T, T, AF.Sqrt)
                P.tt("pool", I, I, XRB[jt], ALU.mult)
                P.tt("pool", T, T, I, ALU.mult)
                P.scan(hs, A_, T, HST[:, jt:jt + 1], ALU.mult, ALU.add)
                P.copy("act", HST[:, jt:jt + 1], hs[:, CT - 1:CT])
                P.tt("dve", GATE[jt], GATE[jt], hs, ALU.mult)
            for nb in range(4):
                w = WB[nw % 2]
                nw += 1
                self.wload(w, self.rg_w_out[j][:, nb * CT:(nb + 1) * CT], RGT, "rg_w_out")
                for mi in range(4):
                    ft = nb * 4 + mi
                    ps = P.ps(npz % 4)
                    npz += 1
                    for k in range(RGT):
                        P.mm(ps, w[:, k, mi * 128:(mi + 1) * 128], GATE[k], start=(k == 0), stop=(k == RGT - 1))
                    P.stt("dve", X[ft], ps, self.mod(li, 2, ft, s), X[ft], ALU.mult, ALU.add)
            self.resid_ln(li, 1, ct, X, self.X1, "X1")
        P.release(m0)

    def bcast_rows(self, dst, src_ap, name):
        n = src_ap.shape[-1]
        self.P.dma("sp", dst, DV(src_ap.broadcast_to([128, n]), name))

    def sgu_layer(self, li, j, xsrc, xname):
        P = self.P
        m0 = P.mark()
        VP = P.sb(f"sgvp{li}", [128, 2, KT], F32)
        P.dma("sp", VP, DV(self.sg_vecp[:, j], "sg_vecp"))
        BIASV = P.alloc([D], F32)
        LNG = P.alloc([D], F32)
        LNB = P.alloc([D], F32)
        BSP = P.alloc([8, 128], F32)
        self.bcast_rows(BIASV, self.sg_rows[j, 0:1, :], "sg_rows")
        self.bcast_rows(LNG, self.sg_rows[j, 1:2, :], "sg_rows")
        self.bcast_rows(LNB, self.sg_rows[j, 2:3, :], "sg_rows")
        self.bcast_rows(BSP.re("p g i -> p (g i)"), self.sg_rows[j, 3:4, 0:1024], "sg_rows")
        WSTf = P.alloc([8, 128], F32)
        WSTb = P.alloc([8, 128], BF16)
        P.dma("sp", WSTf, DV(self.sg_w_spT[j], "sg_w_spT"))
        P.memset("pool", WSTf[64:128, :, 0:64], 0.0)
        P.copy("pool", WSTb, WSTf)
        H = [P.alloc([CT], BF16) for _ in range(KT)]
        WB = [P.alloc([KT, CT], BF16) for _ in range(2)]
        U = [P.alloc([CT], BF16) for _ in range(KT)]
        VT = [P.alloc([D], F32) for _ in range(4)]
        X = [P.alloc([CT], F32, at=VT[0].off + f * 2048) for f in range(KT)]
        VN = [P.alloc([D], BF16) for _ in range(4)]
        XS = [P.alloc([CT], F32) for _ in range(2)]
        T = [P.alloc([CT], F32) for _ in range(2)]
        ST = P.alloc([4, 6], F32)
        MV = P.alloc([4], F32)
        nw = 0
        npz = 0
        for ct in range(self.NCT):
            s = ct // self.CPS
            for k in range(KT):
                xs = XS[k % 2]
                P.dma("sp", xs, self.dv(xsrc[k * 128:(k + 1) * 128, ct * CT:(ct + 1) * CT], xname, ct))
                P.act(H[k], xs, AF.Identity, bias=self.mod(li, 0, k, s), scale=self.mod(li, 1, k, s))
            for nb in range(4):
                w = WB[nw % 2]
                nw += 1
                self.wload(w, self.sg_w_in[j][:, nb * CT:(nb + 1) * CT], KT, "sg_w_in")
                for mi in range(4):
                    ft = nb * 4 + mi
                    ps = P.ps(npz % 4)
                    npz += 1
                    for k in range(KT):
                        P.mm(ps, w[:, k, mi * 128:(mi + 1) * 128], H[k], start=(k == 0), stop=(k == KT - 1))
                    P.act(U[ft], ps, AF.Gelu_apprx_tanh, bias=VP[:, 0, ft:ft + 1])
            for nb in range(4):
                w = WB[nw % 2]
                nw += 1
                self.wload(w, self.sg_w_in[j][:, D + nb * CT:D + (nb + 1) * CT], KT, "sg_w_in")
                for tt in range(4):
                    ps = P.ps(npz % 4)
                    npz += 1
                    for k in range(KT):
                        P.mm(ps, H[k][:, tt * 128:(tt + 1) * 128], w[:, k, :], start=(k == 0), stop=(k == KT - 1))
                    vt = VT[tt][:, nb * CT:(nb + 1) * CT]
                    P.tt("dve", vt, ps, BIASV[:, nb * CT:(nb + 1) * CT], ALU.add)
                    P.act(vt, vt, AF.Gelu_apprx_tanh)
            for tt in range(4):
                P.reduce_add(MV[:, 0:1], VT[tt])
                P.act(VN[tt], VT[tt], AF.Square, accum=MV[:, 1:2])
                P.ts("dve", MV[:, 0:1], MV[:, 0:1], 1.0 / D, ALU.mult)
                P.tt("dve", MV[:, 3:4], MV[:, 0:1], MV[:, 0:1], ALU.mult)
                P.stt("dve", MV[:, 1:2], MV[:, 1:2], 1.0 / D, MV[:, 3:4], ALU.mult, ALU.subtract)
                P.act(MV[:, 2:3], MV[:, 1:2], AF.Sqrt, bias=self.epsc[:, 1:2])
                P.recip(MV[:, 2:3], MV[:, 2:3])
                P.ts("dve", VT[tt], VT[tt], MV[:, 0:1], ALU.subtract, MV[:, 2:3], ALU.mult)
                P.tt("pool", VT[tt], VT[tt], LNG, ALU.mult)
                P.tt("pool", VN[tt], VT[tt], LNB, ALU.add)
            for c in range(KT):
                g = c // 2
                ps = P.ps(4 + c % 2)
                for tt in range(4):
                    P.mm(ps[:, tt * 128:(tt + 1) * 128], VN[tt][:, c * 128:(c + 1) * 128], WSTb[:, g, :])
                t = T[c % 2]
                bsp = BSP[:, g, :]
                P.tt("dve", t.re("p (a i) -> p a i", i=128), ps.re("p (a i) -> p a i", i=128),
                     View(bsp.ap.unsqueeze(1).broadcast_to([128, 4, 128]), bsp.keys), ALU.add)
                P.tt("pool", U[c], U[c], t, ALU.mult)
            for f in range(KT):
                P.dma("sp", X[f], self.dv(xsrc[f * 128:(f + 1) * 128, ct * CT:(ct + 1) * CT], xname, ct))
            for nb in range(4):
                w = WB[nw % 2]
                nw += 1
                self.wload(w, self.sg_w_out[j][:, nb * CT:(nb + 1) * CT], KT, "sg_w_out")
                for mi in range(4):
                    ft = nb * 4 + mi
                    ps = P.ps(npz % 4)
                    npz += 1
                    for k in range(KT):
                        P.mm(ps, w[:, k, mi * 128:(mi + 1) * 128], U[k], start=(k == 0), stop=(k == KT - 1))
                    t = T[ft % 2]
                    P.act(t, ps, AF.Identity, bias=VP[:, 1, ft:ft + 1])
                    P.stt("dve", X[ft], t, self.mod(li, 2, ft, s), X[ft], ALU.mult, ALU.add)
            self.resid_ln(li, 1, ct, X, self.X1, "X1")
        P.release(m0)

    def ml_layer(self, li, j, xsrc, xname):
        P = self.P
        S, NT, NS = self.S, self.CPS, self.NSEQ
        NTT = S // 128
        if not hasattr(self, "ml_scr"):
            self.ml_scr = dict(
                QT=P.dram("mlQT", [D, self.NTOK], BF16), KTs=P.dram("mlKT", [D, self.NTOK], BF16),
                VTK=P.dram("mlVTK", [self.NTOK, D], BF16), XC=P.dram("mlXC", [D, self.NTOK], F32),
                SZ=P.dram("mlSZ", [D, self.NTOK], F32))
            self.ONES256 = P.sb("ones256", [128, 128], F32)
            P.memset("pool", self.ONES256, 1.0 / 256)
            self.ONESB = P.sb("onesb", [128, 128], BF16)
            P.memset("pool", self.ONESB, 1.0)
            self.SELH = P.sb("selh", [8, 8, 128], F32)
            P.copy("pool", self.SELH, View(self.ident.ap[0:8, 0:8].unsqueeze(2).broadcast_to([8, 8, 128]), self.ident.keys))
        scr = self.ml_scr
        m0 = P.mark()
        VP = P.sb(f"mlvp{li}", [128, 7, KT], F32)
        P.dma("sp", VP, DV(self.ml_vecp[:, j], "ml_vecp"))
        BG = P.sb(f"mlbg{li}", [8, 3], F32)
        P.dma("sp", BG[:, 0:2], DV(self.ml_b_gates[:, j], "ml_b_gates"))
        P.ts("dve", BG[:, 2:3], BG[:, 1:2], -1.0, ALU.mult)
        HALO = P.sb(f"mlhalo{li}", [128, KT, 3], F32)
        BD = [P.alloc([KT, 128], BF16) for _ in range(3)]
        for qi in range(3):
            self.P.dma("pool", BD[qi], DV(self.ml_bd[j, qi], "ml_bd"))
        WG = P.alloc([48, 16], BF16)
        P.dma("pool", WG, DV(self.ml_w_gates[j], "ml_w_gates"))
        MASK = P.alloc([4, 512], F32)
        P.dma("sp", MASK, DV(self.maskneg_d, "maskneg"))
        GI = P.alloc([S], F32)
        LF = P.alloc([S], F32)
        BT = P.alloc([S], F32)
        CTK = P.alloc([NTT, 8], F32)
        m1 = P.mark()
        for s in range(NS):
            P.release(m1)
            H = [P.alloc([CT], BF16) for _ in range(KT)]
            WB = [P.alloc([KT, CT], BF16) for _ in range(2)]
            XCB = [P.alloc([CT], BF16) for _ in range(KT)]
            XMB = [P.alloc([CT], BF16) for _ in range(KT)]
            XS = [P.alloc([CT], F32) for _ in range(2)]
            RECB = [P.alloc([CT + 4], F32) for _ in range(2)]
            XCF = [P.alloc([CT], F32) for _ in range(2)]
            QB = [P.alloc([CT], BF16) for _ in range(3)]
            VST = [P.alloc([4, 128], BF16) for _ in range(2)]
            TG_ = [P.alloc([CT], F32) for _ in range(2)]
            nw = 0
            npz = 0
            P.memset("pool", HALO, 0.0)
            for c in range(NT):
                ct = s * NT + c
                for k in range(KT):
                    xs = XS[k % 2]
                    P.dma("sp", xs, self.dv(xsrc[k * 128:(k + 1) * 128, ct * CT:(ct + 1) * CT], xname, ct))
                    P.act(H[k], xs, AF.Identity, bias=self.mod(li, 0, k, s), scale=self.mod(li, 1, k, s))
                for nb in range(8):
                    w = WB[nw % 2]
                    nw += 1
                    self.wload(w, self.ml_w_in[j][:, nb * CT:(nb + 1) * CT], KT, "ml_w_in")
                    for mi in range(4):
                        ft = nb * 4 + mi
                        ps = P.ps(npz % 4)
                        npz += 1
                        for k in range(KT):
                            P.mm(ps, w[:, k, mi * 128:(mi + 1) * 128], H[k], start=(k == 0), stop=(k == KT - 1))
                        if ft < KT:
                            rb, xc = RECB[ft % 2], XCF[ft % 2]
                            P.copy("act", rb[:, 3:3 + CT], ps)
                            P.copy("pool", rb[:, 0:3], HALO[:, ft, :])
                            P.copy("pool", HALO[:, ft, :], rb[:, CT:CT + 3])
                            P.copy("pool", XMB[ft], rb[:, 3:3 + CT])
                            P.act(xc, rb[:, 3:3 + CT], AF.Identity, bias=VP[:, 4, ft:ft + 1], scale=VP[:, 3, ft:ft + 1])
                            for d in (1, 2, 3):
                                P.stt("dve", xc, rb[:, 3 - d:3 - d + CT], VP[:, 3 - d, ft:ft + 1], xc, ALU.mult, ALU.add)
                            P.act(xc, xc, AF.Silu)
                            P.copy("pool", XCB[ft], xc)
                            P.dma("sp", self.dv(scr["XC"][ft * 128:(ft + 1) * 128, ct * CT:(ct + 1) * CT], "mlXC", ct), xc)
                        else:
                            f2 = ft - KT
                            t = TG_[f2 % 2]
                            P.act(t, ps, AF.Sigmoid)
                            P.dma("sp", self.dv(scr["SZ"][f2 * 128:(f2 + 1) * 128, ct * CT:(ct + 1) * CT], "mlSZ", ct), t)
                psGI, psGF = P.ps(6), P.ps(7)
                ng = 0
                for ft in range(KT):
                    for qi, src, dname in ((0, XCB, "QT"), (1, XCB, "KTs"), (2, XMB, None)):
                        ps = P.ps(npz % 4)
                        npz += 1
                        P.mm(ps, BD[qi][:, ft, :], src[ft])
                        qb = QB[qi]
                        P.copy("act" if qi != 1 else "dve", qb, ps)
                        if dname is not None:
                            P.dma("sp", self.dv(scr[dname][ft * 128:(ft + 1) * 128, ct * CT:(ct + 1) * CT], "ml" + dname, ct), qb)
                        P.mm(psGI[0:8], WG[:, qi * KT + ft, 0:8], qb, start=(ng == 0), stop=(ng == 47))
                        P.mm(psGF[0:8], WG[:, qi * KT + ft, 8:16], qb, start=(ng == 0), stop=(ng == 47))
                        ng += 1
                    ps = P.ps(npz % 4)
                    npz += 1
                    for tt in range(4):
                        P.mm(ps[:, tt * 128:(tt + 1) * 128], XMB[ft][:, tt * 128:(tt + 1) * 128], BD[2][:, ft, :])
                    vst = VST[ft % 2]
                    P.copy("dve", vst, ps.re("p (a f) -> p a f", f=128))
                    P.dma("sp", self.dv(scr["VTK"][ct * CT:(ct + 1) * CT, ft * 128:(ft + 1) * 128].rearrange("(t p) f -> p t f", p=128), "mlVTK", ct), vst)
                cs = slice(c * CT, (c + 1) * CT)
                P.act(GI[0:8, cs], psGI[0:8], AF.Identity, bias=BG[:, 0:1])
                P.act(LF[0:8, cs], psGF[0:8], AF.Exp, bias=BG[:, 2:3], scale=-1.0)
                P.act(LF[0:8, cs], LF[0:8, cs], AF.Ln, bias=1.0)
            P.release(m1)
            P.ts("dve", LF[0:8], LF[0:8], -1.0, ALU.mult)
            P.scan(BT[0:8], self.onec[0:8, 0:1].bc([8, S]), LF[0:8], 0.0, ALU.mult, ALU.add)
            P.tt("dve", GI[0:8], GI[0:8], BT[0:8], ALU.subtract)
            pt = P.ps(5)
            for t in range(NTT):
                P.tr(pt[:, t * 8:(t + 1) * 8], GI[0:8, t * 128:(t + 1) * 128], self.ident[0:8, 0:8])
            P.copy("dve", CTK, pt[:, 0:NTT * 8].re("p (t e) -> p t e", e=8))
            GOUT = [P.alloc([S], BF16) for _ in range(KT)]
            m2 = P.mark()
            QH = [[P.alloc([S], BF16) for _ in range(2)] for _ in range(1)]
            KH = [[P.alloc([S], BF16) for _ in range(2)] for _ in range(1)]
            VH = [P.alloc([NTT, 256], BF16) for _ in range(1)]
            BBC = P.alloc([CT], F32)
            DM = [P.alloc([CT], F32) for _ in range(2)]
            PM = [P.alloc([CT], BF16) for _ in range(2)]
            RD = P.alloc([CT], F32)
            HS = [P.alloc([CT], F32) for _ in range(2)]
            SQ = [P.alloc([CT], F32) for _ in range(2)]
            XCt = [P.alloc([CT], F32) for _ in range(2)]
            SZt = [P.alloc([CT], F32) for _ in range(2)]
            MEAN = P.alloc([CT], F32)
            RSTD = P.alloc([CT], F32)
            t0, t1 = s * S, (s + 1) * S
            seqkeys = lambda nm: [("d", nm, s * NT + c) for c in range(NT)]
            nps = 0
            for h in range(8):
                qh, kh, vh = QH[0], KH[0], VH[0]
                for d in range(2):
                    f = 2 * h + d
                    P.dma("sp", qh[d], View(scr["QT"][f * 128:(f + 1) * 128, t0:t1], seqkeys("mlQT")))
                    P.dma("sp", kh[d], View(scr["KTs"][f * 128:(f + 1) * 128, t0:t1], seqkeys("mlKTs")))
                P.dma("sp", vh, View(scr["VTK"][t0:t1, h * 256:(h + 1) * 256].rearrange("(t p) f -> p t f", p=128), seqkeys("mlVTK")))
                for tq in range(NT):
                    ct = s * NT + tq
                    ts_ = slice(tq * CT, (tq + 1) * CT)
                    pb = P.ps(5)
                    P.mm(pb, self.SELH[0:8, h, :], BT[0:8, ts_])
                    P.copy("act", BBC, pb)
                    pn0, pn1, pd = P.ps(2), P.ps(3), P.ps(4)
                    ns = 4 * (tq + 1)
                    for si in range(ns):
                        pS = P.ps(nps % 2)
                        dm, pm = DM[nps % 2], PM[nps % 2]
                        nps += 1
                        ss = slice(si * 128, (si + 1) * 128)
                        P.mm(pS, kh[0][:, ss], qh[0][:, ts_], start=True, stop=False)
                        P.mm(pS, kh[1][:, ss], qh[1][:, ts_], start=False, stop=True)
                        cst = CTK[:, si, h:h + 1]
                        if si >= 4 * tq:
                            P.stt("dve", dm, BBC, cst, MASK[:, si - 4 * tq, :], ALU.add, ALU.add)
                            P.act(dm, dm, AF.Exp)
                        else:
                            P.act(dm, BBC, AF.Exp, bias=cst)
                        P.stt("dve", pm, pS, 1.0 / 16.0, dm, ALU.mult, ALU.mult)
                        P.mm(pn0, vh[:, si, 0:128], pm, start=(si == 0), stop=(si == ns - 1))
                        P.mm(pn1, vh[:, si, 128:256], pm, start=(si == 0), stop=(si == ns - 1))
                        P.mm(pd, self.ONESB, pm, start=(si == 0), stop=(si == ns - 1))
                    P.act(RD, pd, AF.Abs)
                    P.ts("dve", RD, RD, 1.0, ALU.max)
                    P.recip(RD, RD)
                    P.tt("dve", HS[0], pn0, RD, ALU.mult)
                    P.tt("dve", HS[1], pn1, RD, ALU.mult)
                    pm_, pe_ = P.ps(6), P.ps(7)
                    for d in range(2):
                        P.mm(pm_, self.ONES256, HS[d], start=(d == 0), stop=(d == 1))
                    for d in range(2):
                        P.act(SQ[d], HS[d], AF.Square)
                        P.mm(pe_, self.ONES256, SQ[d], start=(d == 0), stop=(d == 1))
                    P.copy("act", MEAN, pm_)
                    P.tt("pool", SQ[0], MEAN, MEAN, ALU.mult)
                    P.tt("dve", RSTD, pe_, SQ[0], ALU.subtract)
                    P.act(RSTD, RSTD, AF.Sqrt, bias=self.epsc[:, 1:2])
                    P.recip(RSTD, RSTD)
                    for d in range(2):
                        f = 2 * h + d
                        P.dma("sp", XCt[d], self.dv(scr["XC"][f * 128:(f + 1) * 128, ct * CT:(ct + 1) * CT], "mlXC", ct))
                        P.dma("sp", SZt[d], self.dv(scr["SZ"][f * 128:(f + 1) * 128, ct * CT:(ct + 1) * CT], "mlSZ", ct))
                        P.tt("dve", HS[d], HS[d], MEAN, ALU.subtract)
                        P.tt("pool", HS[d], HS[d], RSTD, ALU.mult)
                        P.ts("pool", HS[d], HS[d], VP[:, 6, f:f + 1], ALU.mult)
                        P.stt("dve", HS[d], XCt[d], VP[:, 5, f:f + 1], HS[d], ALU.mult, ALU.add)
                        P.tt("pool", GOUT[f][:, ts_], HS[d], SZt[d], ALU.mult)
            P.release(m2)
            X = [P.alloc([CT], F32) for _ in range(KT)]
            WB = [P.alloc([KT, CT], BF16) for _ in range(2)]
            for c in range(NT):
                ct = s * NT + c
                ts_ = slice(c * CT, (c + 1) * CT)
                self.load_x_tiles(xsrc, xname, ct, X)
                for nb in range(4):
                    w = WB[nw % 2]
                    nw += 1
                    self.wload(w, self.ml_w_out[j][:, nb * CT:(nb + 1) * CT], KT, "ml_w_out")
                    for mi in range(4):
                        ft = nb * 4 + mi
                        ps = P.ps(npz % 4)
                        npz += 1
                        for k in range(KT):
                            P.mm(ps, w[:, k, mi * 128:(mi + 1) * 128], GOUT[k][:, ts_], start=(k == 0), stop=(k == KT - 1))
                        P.stt("dve", X[ft], ps, self.mod(li, 2, ft, s), X[ft], ALU.mult, ALU.add)
                self.resid_ln(li, 1, ct, X, self.X1, "X1")
        P.release(m0)


def _pp(v):
    v = np.asarray(v, np.float32)
    sh = v.shape
    v = v.reshape(sh[:-1] + (sh[-1] // 128, 128))
    return np.ascontiguousarray(np.moveaxis(v, -1, 0))


def rg_slab_tiles(j):
    b0, b1 = (128 * j) // 160, (128 * j + 127) // 160
    i0, i1 = (160 * b0) // 128, (160 * b1 + 159) // 128
    return list(range(i0, i1 + 1))


class _Sel:
    def __init__(self, inp, sel):
        self.inp, self.sel = inp, sel

    def __getitem__(self, name):
        return _SelArr(self.inp[name], self.sel[name])


class _SelArr:
    def __init__(self, a, idx):
        self.a, self.idx = a, idx

    def __getitem__(self, key):
        if isinstance(key, (int, np.integer)):
            return np.asarray(self.a[self.idx[key]], np.float32)
        g = np.stack([np.asarray(self.a[i], np.float32) for i in self.idx], axis=0)
        if isinstance(key, tuple):
            return g[(slice(None),) + key[1:]]
        return g


def prep_shared(inp_raw, layers, li_ids=None):
    L = len(layers)
    li_ids = list(range(L)) if li_ids is None else list(li_ids)
    jsel = {k: [j for kk, j in layers if kk == k] or [0] for k in (0, 1, 2)}
    sel = {}
    for name in inp_raw:
        if name.startswith("rg_"):
            sel[name] = jsel[0]
        elif name.startswith("sg_"):
            sel[name] = jsel[1]
        elif name.startswith("ml_"):
            sel[name] = jsel[2]
        else:
            sel[name] = li_ids
    inp = _Sel(inp_raw, sel)
    out = {}
    f = lambda a: np.ascontiguousarray(np.asarray(a, np.float32))
    out["ada_w"] = f(inp["ada_w"][:L])
    out["ada_bT"] = _pp(inp["ada_b"][:L])
    lnp = np.stack([inp["ln1_g"][:L], inp["ln1_b"][:L], inp["ln2_g"][:L], inp["ln2_b"][:L]], axis=1)
    out["lnp"] = _pp(lnp)
    out["ident"] = np.eye(128, dtype=np.float32)
    mk = np.zeros((128, 4, 512), np.float32)
    for o in range(4):
        sidx = o * 128 + np.arange(128)[:, None]
        tidx = np.arange(512)[None, :]
        mk[:, o, :] = np.where(sidx <= tidx, 0.0, -30000.0)
    out["maskneg"] = mk
    nA = max(1, sum(1 for k, _ in layers if k == 0))
    nB = max(1, sum(1 for k, _ in layers if k == 1))
    nC = max(1, sum(1 for k, _ in layers if k == 2))
    out["rg_w_in"] = f(inp["rg_w_in"][:nA])
    vec = np.concatenate([inp["rg_conv_w"][:nA], inp["rg_conv_b"][:nA, None], inp["rg_b_rgate"][:nA, None],
                          inp["rg_b_igate"][:nA, None], inp["rg_lam"][:nA, None], np.zeros_like(inp["rg_lam"][:nA, None])], axis=1)
    out["rg_vec"] = _pp(vec)
    slabs = np.zeros((nA, RGT, 128, 2, 4, 128), np.float32)
    for a in range(nA):
        for gi, wname in enumerate(("rg_w_rgate", "rg_w_igate")):
            w = np.asarray(inp[wname][a], np.float32)
            for jt in range(RGT):
                for si, it in enumerate(rg_slab_tiles(jt)):
                    for n in range(16):
                        r0, r1 = max(it * 128, n * 160), min(it * 128 + 128, n * 160 + 160)
                        c0, c1 = max(jt * 128, n * 160), min(jt * 128 + 128, n * 160 + 160)
                        if r0 < r1 and c0 < c1:
                            slabs[a, jt, r0 - it * 128:r1 - it * 128, gi, si, c0 - jt * 128:c1 - jt * 128] = \
                                w[n, r0 - n * 160:r1 - n * 160, c0 - n * 160:c1 - n * 160]
    out["rg_slabs"] = slabs
    out["rg_w_out"] = f(inp["rg_w_out"][:nA])
    out["sg_w_in"] = f(inp["sg_w_in"][:nB])
    out["sg_vecp"] = _pp(np.stack([inp["sg_b_in"][:nB, :D], inp["sg_b_out"][:nB]], axis=1))
    bsp = np.zeros((nB, D), np.float32)
    bsp[:, :1024] = np.asarray(inp["sg_b_sp"][:nB]).reshape(nB, 1024)
    out["sg_rows"] = f(np.stack([inp["sg_b_in"][:nB, D:], inp["sg_ln_g"][:nB], inp["sg_ln_b"][:nB], bsp], axis=1))
    out["sg_w_spT"] = f(np.transpose(np.asarray(inp["sg_w_sp"][:nB]), (0, 3, 1, 2)))
    out["sg_w_out"] = f(inp["sg_w_out"][:nB])
    out["ml_w_in"] = f(inp["ml_w_in"][:nC])
    out["ml_vecp"] = _pp(np.concatenate([inp["ml_conv_w"][:nC], inp["ml_conv_b"][:nC, None], inp["ml_skip"][:nC, None],
                                         inp["ml_norm_g"][:nC, None]], axis=1))
    bd = np.zeros((nC, 3, 128, KT, 128), np.float32)
    for a in range(nC):
        for qi, wname in enumerate(("ml_w_q", "ml_w_k", "ml_w_v")):
            w = np.asarray(inp[wname][a], np.float32)
            for t in range(KT):
                for b in range(32):
                    bd[a, qi, b * 4:b * 4 + 4, t, b * 4:b * 4 + 4] = w[t * 32 + b]
    out["ml_bd"] = bd
    wg = np.asarray(inp["ml_w_gates"][:nC], np.float32).reshape(nC, 48, 128, 16)
    out["ml_w_gates"] = f(np.transpose(wg, (0, 2, 1, 3)))
    out["ml_b_gates"] = f(np.transpose(np.asarray(inp["ml_b_gates"][:nC]).reshape(nC, 2, 8), (2, 0, 1)))
    out["ml_w_out"] = f(inp["ml_w_out"][:nC])
    wr = np.asarray(inp["moe_w_router"][:L], np.float32).reshape(L, KT, 128, NE)
    out["moe_w_router"] = f(np.transpose(wr, (0, 2, 1, 3)))
    out["moe_b_router"] = f(np.asarray(inp["moe_b_router"][:L]).reshape(L, NE, 1))
    out["moe_w_gate_up"] = f(inp["moe_w_gate_up"][:L])
    out["moe_b_gu"] = _pp(inp["moe_b_gate_up"][:L])
    out["moe_w_down"] = f(inp["moe_w_down"][:L])
    out["moe_b_down"] = f(inp["moe_b_down"][:L])
    return out


def prep_core(x, c, b0, nseq):
    xs = np.asarray(x[b0:b0 + nseq], np.float32)
    S = xs.shape[1]
    xT = np.ascontiguousarray(xs.reshape(nseq * S, D).T)
    cT = _pp(np.asarray(c[b0:b0 + nseq], np.float32))
    cT = np.ascontiguousarray(np.transpose(cT, (0, 2, 1)))
    return {"xT": xT, "cT": cT}


_CACHE = {}


def run(inp, layers, nseq, ncores, trace=False, li_ids=None):
    x = np.asarray(inp["x"])
    B, S, _ = x.shape
    cnt = {0: 0, 1: 0, 2: 0}
    local = []
    for k, j in layers:
        local.append((k, cnt[k]))
        cnt[k] += 1
    key = (S, nseq, tuple(local))
    if key not in _CACHE:
        _CACHE[key] = MK(NSEQ=nseq, S=S, layers=local)
    mk = _CACHE[key]
    shared = prep_shared(inp, layers, li_ids)
    in_maps = []
    for r in range(ncores):
        d = dict(shared)
        d.update(prep_core(x, inp["c"], r * nseq, nseq))
        for n, (shape, _) in mk.inputs.items():
            assert tuple(d[n].shape) == shape, (n, d[n].shape, shape)
        in_maps.append({n: d[n] for n in mk.inputs})
    res = run_bass_kernel_spmd(mk.nc, in_maps, core_ids=list(range(ncores)), trace=trace)
    outs = []
    for r in range(ncores):
        oT = np.asarray(res.results[r]["outT"])
        outs.append(oT.T.reshape(nseq, S, D))
    return np.concatenate(outs, axis=0), res


LAUNCHES = [([(0, 0), (1, 0)], [0, 1]), ([(2, 0), (0, 1)], [2, 3])]


def kernel(**inputs):
    inp = dict(inputs)
    out = None
    for layers, li_ids in LAUNCHES:
        out, _ = run(inp, layers, nseq=2, ncores=8, li_ids=li_ids)
        inp["x"] = out
    return out.astype(np.float32)
```

```python
import numpy as np
from contextlib import ExitStack
import concourse.bass as bass
import concourse.mybir as mybir
from concourse.bass_utils import run_bass_kernel_spmd

F32 = mybir.dt.float32
BF16 = mybir.dt.bfloat16
AF = mybir.ActivationFunctionType
ALU = mybir.AluOpType

D = 2048
KT = 16
CT = 512
RGW = 2560
RGT = 20
NE = 32
DE = 512
ALPHA = 8 ** 0.25
LN_EPS = 1e-5
SAME_ENG_SYNC = True


def dsize(dt):
    return 2 if dt == BF16 else 4


class View:
    __slots__ = ("ap", "keys", "off")

    def __init__(self, ap, keys, off=None):
        self.ap = ap
        self.keys = keys
        self.off = off

    def __getitem__(self, idx):
        return View(self.ap[idx], self.keys)

    def re(self, pat, **kw):
        return View(self.ap.rearrange(pat, **kw), self.keys)

    def bc(self, shape):
        return View(self.ap.broadcast_to(shape), self.keys)


class Prog:
    ENGS = ("pe", "act", "dve", "pool", "sp")
    NDMA = {"sp": 12, "pool": 12}

    def __init__(self, nc, arena_kib=168):
        self.nc = nc
        self.es = ExitStack()
        self.streams = {e: [] for e in self.ENGS}
        self.cnt = {e: 0 for e in self.ENGS}
        self.clock = {e: {} for e in self.ENGS}
        self.dcnt = {}
        self.drr = {q: 0 for q in self.NDMA}
        self.lw = {}
        self.rd = {}
        self.arena_bytes = arena_kib * 1024
        self.arena = self.es.enter_context(nc.sbuf_tensor("arena", [128, self.arena_bytes // 4], F32))
        self.top = 0
        self.psb = [self.es.enter_context(nc.psum_tensor(f"psb{i}", [128, 512], F32)) for i in range(8)]
        self.ninst = 0

    def alloc(self, shape, dt, at=None):
        n = int(np.prod(shape)) * dsize(dt)
        nr = (n + 1023) // 1024 * 1024
        if at is None:
            off = self.top
            self.top += nr
            assert self.top <= self.arena_bytes, f"arena overflow {self.top}"
        else:
            off = at
            assert off % 1024 == 0 and off + nr <= self.arena_bytes
        ap = self.arena[:, off // 4:(off + n + 3) // 4]
        if dt != F32:
            ap = ap.bitcast(dt)
        if len(shape) == 2:
            ap = ap.rearrange("p (a b) -> p a b", a=shape[0])
        elif len(shape) == 3:
            ap = ap.rearrange("p (a b c) -> p a b c", a=shape[0], b=shape[1])
        return View(ap, [("a", p) for p in range(off // 1024, (off + nr) // 1024)], off)

    def mark(self):
        return self.top

    def release(self, m):
        self.top = m

    def sb(self, name, shape, dt):
        t = self.es.enter_context(self.nc.sbuf_tensor(name, list(shape), dt))
        return View(t[:], [("t", name)])

    def ps(self, b, dt=F32):
        return View(self.psb[b][:], [("ps", b)])

    def dram(self, name, shape, dt, kind="Internal"):
        t = self.nc.dram_tensor(name, list(shape), dt, kind=kind).ap()
        return t

    def _deps(self, reads, writes):
        deps = []
        for v in reads:
            for k in v.keys:
                e = self.lw.get(k)
                if e is not None:
                    deps.append(e)
        for v in writes:
            for k in v.keys:
                e = self.lw.get(k)
                if e is not None:
                    deps.append(e)
                r = self.rd.get(k)
                if r:
                    deps.extend(r)
        return deps

    def _waits(self, eng, deps):
        clk = self.clock[eng]
        need = {}
        for (sem, val, peng) in deps:
            if peng == eng and (eng == "pe" or not SAME_ENG_SYNC):
                continue
            if clk.get(sem, 0) >= val:
                continue
            if need.get(sem, 0) < val:
                need[sem] = val
        for sem, val in need.items():
            clk[sem] = val
        return list(need.items())

    def _record(self, ev, reads, writes):
        for v in writes:
            for k in v.keys:
                self.lw[k] = ev
                self.rd[k] = []
        for v in reads:
            for k in v.keys:
                lst = self.rd.setdefault(k, [])
                lst[:] = [x for x in lst if x[0] != ev[0]]
                lst.append(ev)

    def op(self, eng, fn, reads, writes):
        reads = [v for v in reads if isinstance(v, View)]
        waits = self._waits(eng, self._deps(reads, writes))
        self.cnt[eng] += 1
        ev = (eng, self.cnt[eng], eng)
        self.streams[eng].append((waits, fn, (eng, 1)))
        self._record(ev, reads, writes)
        self.ninst += 1
        return ev

    def dma(self, q, out, in_):
        i = self.drr[q]
        self.drr[q] = (i + 1) % self.NDMA[q]
        sem = f"d{q}{i}"
        c = self.dcnt.get(sem, 0)
        deps = self._deps([in_], [out])
        if c > 0:
            deps.append((sem, 16 * c, "dma"))
        waits = self._waits(q, deps)
        self.dcnt[sem] = c + 1
        ev = (sem, 16 * (c + 1), "dma")
        oap, iap = out.ap, in_.ap
        self.streams[q].append((waits, lambda e: e.dma_start(out=oap, in_=iap), (sem, 16)))
        self._record(ev, [in_], [out])
        self.ninst += 1
        return ev

    def wait_all(self, eng, evs):
        waits = self._waits(eng, evs)
        self.streams[eng].append((waits, None, None))

    def emit(self):
        nc = self.nc
        names = set(self.ENGS) | set(self.dcnt.keys())
        sems = {n: self.es.enter_context(nc.semaphore(n)) for n in sorted(names)}
        block = self.es.enter_context(nc.Block())

        def runner(en):
            def f(eng):
                for waits, fn, inc in self.streams[en]:
                    if fn is None:
                        for (s, v) in waits:
                            eng.wait_ge(sems[s], v)
                        continue
                    for (s, v) in waits[1:]:
                        eng.wait_ge(sems[s], v)
                    ins = fn(eng)
                    if waits:
                        ins._wait_ge(sems[waits[0][0]], waits[0][1])
                    ins.then_inc(sems[inc[0]], inc[1])
            return f
        block.tensor(runner("pe"))
        block.scalar(runner("act"))
        block.vector(runner("dve"))
        block.gpsimd(runner("pool"))
        block.sync(runner("sp"))
        self.es.close()

    @staticmethod
    def _a(x):
        return x.ap if isinstance(x, View) else x

    def mm(self, out, lhsT, rhs, start=True, stop=True):
        o, l, r = out.ap, lhsT.ap, rhs.ap
        return self.op("pe", lambda e: e.matmul(o, lhsT=l, rhs=r, start=start, stop=stop), [lhsT, rhs], [out])

    def tr(self, out, in_, ident):
        o, i, d = out.ap, in_.ap, ident.ap
        return self.op("pe", lambda e: e.transpose(o, i, d), [in_, ident], [out])

    def act(self, out, in_, func, bias=0.0, scale=1.0, accum=None):
        o, i, b, s = out.ap, in_.ap, self._a(bias), self._a(scale)
        if accum is not None:
            a = accum.ap
            return self.op("act", lambda e: e.activation(out=o, in_=i, func=func, bias=b, scale=s, accum_out=a), [in_, bias, scale], [out, accum])
        return self.op("act", lambda e: e.activation(out=o, in_=i, func=func, bias=b, scale=s), [in_, bias, scale], [out])

    def reduce_add(self, out, in_):
        o, i = out.ap, in_.ap
        return self.op("dve", lambda e: e.tensor_reduce(out=o, in_=i, axis=mybir.AxisListType.X, op=ALU.add), [in_], [out])

    def tt(self, eng, out, in0, in1, op):
        o, a, b = out.ap, in0.ap, in1.ap
        return self.op(eng, lambda e: e.tensor_tensor(out=o, in0=a, in1=b, op=op), [in0, in1], [out])

    def ts(self, eng, out, in0, s1, op0, s2=None, op1=None):
        o, a, x1, x2 = out.ap, in0.ap, self._a(s1), self._a(s2)
        if op1 is None:
            return self.op(eng, lambda e: e.tensor_scalar(out=o, in0=a, scalar1=x1, scalar2=None, op0=op0), [in0, s1], [out])
        return self.op(eng, lambda e: e.tensor_scalar(out=o, in0=a, scalar1=x1, scalar2=x2, op0=op0, op1=op1), [in0, s1, s2], [out])

    def stt(self, eng, out, in0, scalar, in1, op0, op1):
        o, a, s, b = out.ap, in0.ap, self._a(scalar), in1.ap
        return self.op(eng, lambda e: e.scalar_tensor_tensor(out=o, in0=a, scalar=s, in1=b, op0=op0, op1=op1), [in0, scalar, in1], [out])

    def copy(self, eng, out, in_):
        o, i = out.ap, in_.ap
        if eng == "act":
            return self.op("act", lambda e: e.copy(out=o, in_=i), [in_], [out])
        return self.op(eng, lambda e: e.tensor_copy(out=o, in_=i), [in_], [out])

    def memset(self, eng, out, val):
        o = out.ap
        return self.op(eng, lambda e: e.memset(o, val), [], [out])

    def scan(self, out, d0, d1, init, op0, op1):
        o, a, b, i = out.ap, d0.ap, d1.ap, self._a(init)
        return self.op("dve", lambda e: e.tensor_tensor_scan(out=o, data0=a, data1=b, initial=i, op0=op0, op1=op1), [d0, d1, init], [out])

    def recip(self, out, in_):
        o, i = out.ap, in_.ap
        return self.op("dve", lambda e: e.reciprocal(out=o, in_=i), [in_], [out])

    def max8(self, out, in_):
        o, i = out.ap, in_.ap
        return self.op("dve", lambda e: e.max(out=o, in_=i), [in_], [out])

    def bn_stats(self, out, in_):
        o, i = out.ap, in_.ap
        return self.op("dve", lambda e: e.bn_stats(out=o, in_=i), [in_], [out])

    def bn_aggr(self, out, in_):
        o, i = out.ap, in_.ap
        return self.op("dve", lambda e: e.bn_aggr(out=o, in_=i), [in_], [out])


def DV(ap, name, region=0):
    return View(ap, [("d", name, region)])


class MK:
    def __init__(self, NSEQ=2, S=2048, layers=None, dbg=None):
        self.NSEQ, self.S = NSEQ, S
        self.NTOK = NSEQ * S
        self.NCT = self.NTOK // CT
        self.CPS = S // CT
        self.layers = layers if layers is not None else [(0, 0), (1, 0), (2, 0), (0, 1)]
        self.L = len(self.layers)
        self.nA = sum(1 for k, _ in self.layers if k == 0)
        self.nB = sum(1 for k, _ in self.layers if k == 1)
        self.nC = sum(1 for k, _ in self.layers if k == 2)
        self.dbg = dbg
        nc = self.nc = bass.Bass("TRN2", target_bir_lowering=False)
        self.P = Prog(nc, arena_kib=176)
        self.inputs = {}
        self.build()

    def inp(self, name, shape, dt=F32):
        ap = self.nc.dram_tensor(name, list(shape), dt, kind="ExternalInput").ap()
        self.inputs[name] = (tuple(shape), ap)
        return ap

    def build(self):
        P, L, NT = self.P, self.L, self.NTOK
        nA, nB, nC = max(self.nA, 1), max(self.nB, 1), max(self.nC, 1)
        self.xT = self.inp("xT", [D, NT])
        self.cT = self.inp("cT", [128, KT, self.NSEQ])
        self.ada_w = self.inp("ada_w", [L, D, 6 * D])
        self.ada_bT = self.inp("ada_bT", [128, L, 96])
        self.lnp = self.inp("lnp", [128, L, 4, KT])
        self.ident_d = self.inp("ident", [128, 128])
        self.maskneg_d = self.inp("maskneg", [128, 4, 512])
        self.rg_w_in = self.inp("rg_w_in", [nA, D, 2 * RGW])
        self.rg_vec = self.inp("rg_vec", [128, nA, 9, RGT])
        self.rg_slabs = self.inp("rg_slabs", [nA, RGT, 128, 2, 4, 128])
        self.rg_w_out = self.inp("rg_w_out", [nA, RGW, D])
        self.sg_w_in = self.inp("sg_w_in", [nB, D, 2 * D])
        self.sg_vecp = self.inp("sg_vecp", [128, nB, 2, KT])
        self.sg_rows = self.inp("sg_rows", [nB, 4, D])
        self.sg_w_spT = self.inp("sg_w_spT", [nB, 128, 8, 128])
        self.sg_w_out = self.inp("sg_w_out", [nB, D, D])
        self.ml_w_in = self.inp("ml_w_in", [nC, D, 2 * D])
        self.ml_vecp = self.inp("ml_vecp", [128, nC, 7, KT])
        self.ml_bd = self.inp("ml_bd", [nC, 3, 128, KT, 128])
        self.ml_w_gates = self.inp("ml_w_gates", [nC, 128, 48, 16])
        self.ml_b_gates = self.inp("ml_b_gates", [8, nC, 2])
        self.ml_w_out = self.inp("ml_w_out", [nC, D, D])
        self.moe_w_router = self.inp("moe_w_router", [L, 128, KT, NE])
        self.moe_b_router = self.inp("moe_b_router", [L, NE, 1])
        self.moe_w_gate_up = self.inp("moe_w_gate_up", [L, NE, D, 2 * DE])
        self.moe_b_gu = self.inp("moe_b_gu", [128, L, NE, 8])
        self.moe_w_down = self.inp("moe_w_down", [L, NE, DE, D])
        self.moe_b_down = self.inp("moe_b_down", [L, NE, D])
        self.outT = self.nc.dram_tensor("outT", [D, NT], F32, kind="ExternalOutput").ap()
        self.X1 = P.dram("X1", [D, NT], F32)
        self.X2 = P.dram("X2", [D, NT], F32)
        self.YT = P.dram("YT", [D, NT], F32)

        self.setup_consts()
        self.compute_mods()
        xin = self.xT
        xin_name = "xT"
        for li, (kind, j) in enumerate(self.layers):
            last = li == self.L - 1
            if kind == 0:
                self.rg_layer(li, j, xin, xin_name)
            elif kind == 1:
                self.sgu_layer(li, j, xin, xin_name)
            else:
                self.ml_layer(li, j, xin, xin_name)
            dst, dname = (self.outT, "outT") if last else (self.X2, "X2")
            self.moe_layer(li, dst, dname)
            xin, xin_name = self.X2, "X2"
        evs = []
        for k, e in P.lw.items():
            if k[0] == "d" and k[1] == "outT":
                evs.append(e)
        P.wait_all("sp", evs)
        P.emit()

    def dv(self, ap, name, ct):
        return DV(ap, name, ct)

    def setup_consts(self):
        P = self.P
        self.ident = P.sb("identc", [128, 128], F32)
        P.dma("sp", self.ident, DV(self.ident_d, "ident"))
        self.onesD = P.sb("onesD", [128, 128], F32)
        P.memset("pool", self.onesD, 1.0 / D)
        self.onec = P.sb("onec", [128, 1], F32)
        P.memset("pool", self.onec, 1.0)
        self.epsc = P.sb("epsc", [128, 2], F32)
        P.memset("pool", self.epsc[:, 0:1], LN_EPS / (ALPHA * ALPHA))
        P.memset("pool", self.epsc[:, 1:2], LN_EPS)
        self.MOD = P.sb("MOD", [128, self.L, 6, KT, self.NSEQ], F32)
        self.LNP = P.sb("LNP", [128, self.L, 4, KT], F32)
        P.dma("sp", self.LNP, DV(self.lnp, "lnp"))
        self.SEL = P.sb("SEL", [32, 2, 128], F32)
        self.lnmean = P.sb("lnmean", [128, 512], F32)
        self.lnrstd = P.sb("lnrstd", [128, 512], F32)
        self.lntmp = P.sb("lntmp", [128, 2, 512], F32)

    def compute_mods(self):
        P, L, NS = self.P, self.L, self.NSEQ
        m = P.mark()
        cT = P.alloc([KT, NS], F32)
        P.dma("sp", cT, DV(self.cT, "cT"))
        cond = P.alloc([KT, NS], F32)
        P.act(cond, cT, AF.Silu)
        abT = P.alloc([L, 96], F32)
        P.dma("sp", abT, DV(self.ada_bT, "ada_bT"))
        wb = [P.alloc([D], F32) for _ in range(3)]
        acc = P.alloc([KT, NS], F32)
        n = 0
        for l in range(L):
            for c6 in range(6):
                for k in range(KT):
                    w = wb[n % 3]
                    n += 1
                    P.dma("sp", w, DV(self.ada_w[l, k * 128:(k + 1) * 128, c6 * D:(c6 + 1) * D], "ada_w"))
                    ps = P.ps(k % 2)
                    for f in range(KT):
                        P.mm(ps[:, f * NS:(f + 1) * NS], w[:, f * 128:(f + 1) * 128], cond[:, k, :])
                    pv = ps[:, 0:KT * NS].re("p (f s) -> p f s", s=NS)
                    if k == 0:
                        P.copy("dve", acc, pv)
                    else:
                        P.tt("dve", acc, acc, pv, ALU.add)
                bias = abT[:, l, c6 * KT:(c6 + 1) * KT]
                bb = View(bias.ap.unsqueeze(2).broadcast_to([128, KT, NS]), bias.keys)
                dst = self.MOD[:, l, c6]
                if c6 in (1, 4):
                    P.stt("dve", dst, acc, 1.0, bb, ALU.add, ALU.add)
                elif c6 in (2, 5):
                    P.tt("dve", dst, acc, bb, ALU.add)
                    P.ts("dve", dst, dst, 1.0, ALU.add, 1.0 / ALPHA, ALU.mult)
                else:
                    P.tt("dve", dst, acc, bb, ALU.add)
        P.release(m)

    def mod(self, l, c6, k, s):
        return self.MOD[:, l, c6, k, s:s + 1]

    def resid_ln(self, li, which, ct, V, dst, dname):
        P = self.P
        psm, pse = P.ps(6), P.ps(7)
        for k in range(KT):
            P.mm(psm, self.onesD, V[k], start=(k == 0), stop=(k == KT - 1))
        for k in range(KT):
            sq = self.lntmp[:, k % 2]
            P.act(sq, V[k], AF.Square)
            P.mm(pse, self.onesD, sq, start=(k == 0), stop=(k == KT - 1))
        mean, rstd = self.lnmean, self.lnrstd
        P.copy("act", mean, psm)
        m2 = self.lntmp[:, 0]
        P.tt("pool", m2, mean, mean, ALU.mult)
        P.tt("dve", rstd, pse, m2, ALU.subtract)
        P.act(rstd, rstd, AF.Sqrt, bias=self.epsc[:, 0:1])
        P.recip(rstd, rstd)
        gi = 0 if which == 1 else 2
        for k in range(KT):
            P.tt("dve", V[k], V[k], mean, ALU.subtract)
            P.tt("pool", V[k], V[k], rstd, ALU.mult)
            P.act(V[k], V[k], AF.Identity, bias=self.LNP[:, li, gi + 1, k:k + 1], scale=self.LNP[:, li, gi, k:k + 1])
            P.dma("sp", self.dv(dst[k * 128:(k + 1) * 128, ct * CT:(ct + 1) * CT], dname, ct), V[k])

    def load_x_tiles(self, src, sname, ct, X):
        for k in range(KT):
            self.P.dma("sp", X[k], self.dv(src[k * 128:(k + 1) * 128, ct * CT:(ct + 1) * CT], sname, ct))

    def modulate(self, li, which, ct, X, H):
        s = ct // self.CPS
        c0 = 0 if which == 1 else 3
        for k in range(KT):
            self.P.act(H[k], X[k], AF.Identity, bias=self.mod(li, c0, k, s), scale=self.mod(li, c0 + 1, k, s))

    def stream(self, WB, items, body):
        def ld(i):
            src, kt, name = items[i]
            self.wload(WB[i % 2][:, 0:kt], src, kt, name)
        ld(0)
        for i in range(len(items)):
            if i + 1 < len(items):
                ld(i + 1)
            body(i, WB[i % 2])

    def wload(self, buf, src_ap, kt, name):
        step = 8
        for k0 in range(0, kt, step):
            k1 = min(kt, k0 + step)
            self.P.dma("pool", buf[:, k0:k1], DV(src_ap[k0 * 128:k1 * 128, :].rearrange("(k p) n -> p k n", p=128), name))

    def moe_layer(self, li, dst, dname):
        P = self.P
        TG = 2 * CT
        for g in range(self.NTOK // TG):
            m0 = P.mark()
            cts = (2 * g, 2 * g + 1)
            H2 = [P.alloc([TG], BF16) for _ in range(KT)]
            ACC = [[P.alloc([CT], F32) for _ in range(2)] for _ in range(KT)]
            CBT = P.alloc([TG], F32)
            BGU = P.alloc([NE, 8], F32)
            P.dma("sp", BGU, DV(self.moe_b_gu[:, li], "moe_b_gu"))
            m1 = P.mark()
            WR = P.alloc([KT, NE], F32)
            P.dma("sp", WR, DV(self.moe_w_router[li], "moe_w_router"))
            BR = P.alloc([1], F32)
            P.dma("sp", BR[0:32], DV(self.moe_b_router[li], "moe_b_router"))
            BD = P.alloc([D], F32)
            P.dma("sp", BD[0:32], DV(self.moe_b_down[li], "moe_b_down"))
            XS = [P.alloc([TG], F32) for _ in range(2)]
            HF = [P.alloc([TG], F32) for _ in range(2)]
            for k in range(KT):
                xs, hf = XS[k % 2], HF[k % 2]
                for c in range(2):
                    ct = cts[c]
                    s = ct // self.CPS
                    P.dma("sp", xs[:, c * CT:(c + 1) * CT], self.dv(self.X1[k * 128:(k + 1) * 128, ct * CT:(ct + 1) * CT], "X1", ct))
                    P.act(hf[:, c * CT:(c + 1) * CT], xs[:, c * CT:(c + 1) * CT], AF.Identity,
                          bias=self.mod(li, 3, k, s), scale=self.mod(li, 4, k, s))
                P.copy("dve", H2[k], hf)
                for c in range(2):
                    P.mm(P.ps(c)[0:32], WR[:, k, :], hf[:, c * CT:(c + 1) * CT], start=(k == 0), stop=(k == KT - 1))
            LT = P.alloc([TG], F32)
            for c in range(2):
                P.act(LT[0:32, c * CT:(c + 1) * CT], P.ps(c)[0:32], AF.Identity, bias=BR[0:32, 0:1])
            NTT = TG // 128
            LG = P.alloc([NTT, NE], F32)
            pt = P.ps(2)
            for tt in range(NTT):
                P.tr(pt[:, tt * NE:(tt + 1) * NE], LT[0:32, tt * 128:(tt + 1) * 128], self.ident[0:32, 0:32])
            P.copy("dve", LG, pt[:, 0:NTT * NE].re("p (t e) -> p t e", e=NE))
            M8 = P.alloc([NTT, 8], F32)
            for tt in range(NTT):
                P.max8(M8[:, tt, :], LG[:, tt, :])
            MSK = P.alloc([NTT, NE], F32)
            EX = P.alloc([NTT, NE], F32)
            NM = P.alloc([NTT], F32)
            P.ts("dve", NM, M8[:, :, 0], -1.0, ALU.mult)
            for tt in range(NTT):
                P.ts("dve", MSK[:, tt, :], LG[:, tt, :], M8[:, tt, 3:4], ALU.is_ge)
                P.act(EX[:, tt, :], LG[:, tt, :], AF.Exp, bias=NM[:, tt:tt + 1])
            P.tt("dve", EX, EX, MSK, ALU.mult)
            SM = P.alloc([NTT], F32)
            sm_o, ex_i = SM.ap, EX.ap
            P.op("dve", lambda e: e.tensor_reduce(out=sm_o, in_=ex_i, axis=mybir.AxisListType.X, op=ALU.add), [EX], [SM])
            P.recip(SM, SM)
            P.tt("dve", EX, EX, View(SM.ap.unsqueeze(2).broadcast_to([128, NTT, NE]), SM.keys), ALU.mult)
            for tt in range(NTT):
                c = tt // 4
                P.tr(P.ps(c)[0:32, (tt % 4) * 128:(tt % 4 + 1) * 128], EX[:, tt, :], self.ident)
            for c in range(2):
                P.copy("act", CBT[0:32, c * CT:(c + 1) * CT], P.ps(c)[0:32])
            n = 0
            for f in range(KT):
                for c in range(2):
                    ps = P.ps(4 + n % 2)
                    n += 1
                    P.mm(ps, BD[0:32, f * 128:(f + 1) * 128], CBT[0:32, c * CT:(c + 1) * CT])
                    P.copy("act", ACC[f][c], ps)
            P.release(m1)
            WGU = [P.alloc([KT, 2, 256], BF16) for _ in range(2)]
            WDN = P.alloc([4, D], BF16)
            A = [[P.alloc([CT], BF16) for _ in range(4)] for _ in range(2)]
            CBE = [P.alloc([CT], F32) for _ in range(2)]
            Gb = [P.alloc([CT], F32) for _ in range(2)]
            Ub = [P.alloc([CT], F32) for _ in range(2)]
            Sb = [P.alloc([CT], F32) for _ in range(2)]
            nw = 0
            nm = 0
            nd = 0
            def load_chunk(n):
                e_, mp_ = divmod(n, 2)
                w_ = WGU[n % 2]
                wsrc_ = self.moe_w_gate_up[li, e_]
                for gu in range(2):
                    col0 = gu * DE + mp_ * 256
                    for k0 in (0, 8):
                        P.dma("pool", w_[:, k0:k0 + 8, gu, :],
                              DV(wsrc_[k0 * 128:(k0 + 8) * 128, col0:col0 + 256].rearrange("(k p) n -> p k n", p=128), "moe_w_gate_up"))
            load_chunk(0)
            self.wload(WDN, self.moe_w_down[li, 0], 4, "moe_w_down")
            for e in range(NE):
                wsrc = self.moe_w_gate_up[li, e]
                sel = self.SEL[:, e % 2, :]
                P.copy("pool", sel, View(self.ident.ap[0:32, e:e + 1].broadcast_to([32, 128]), self.ident.keys))
                for c in range(2):
                    ps = P.ps(6)
                    P.mm(ps, sel, CBT[0:32, c * CT:(c + 1) * CT])
                    P.copy("act", CBE[c], ps)
                for mp in range(2):
                    w = WGU[nw % 2]
                    if mp == 1 and e > 0:
                        self.wload(WDN, self.moe_w_down[li, e], 4, "moe_w_down")
                    if nw + 1 < 2 * NE:
                        load_chunk(nw + 1)
                    nw += 1
                    for c in range(2):
                        for mi in range(2):
                            mt = mp * 2 + mi
                            psG, psU = P.ps(nm % 2), P.ps(2 + nm % 2)
                            G, U, S_ = Gb[nm % 2], Ub[nm % 2], Sb[nm % 2]
                            nm += 1
                            for k in range(KT):
                                P.mm(psG, w[:, k, 0, mi * 128:(mi + 1) * 128], H2[k][:, c * CT:(c + 1) * CT], start=(k == 0), stop=(k == KT - 1))
                            for k in range(KT):
                                P.mm(psU, w[:, k, 1, mi * 128:(mi + 1) * 128], H2[k][:, c * CT:(c + 1) * CT], start=(k == 0), stop=(k == KT - 1))
                            P.ts("dve", G, psG, BGU[:, e, mt:mt + 1], ALU.add, 7.0, ALU.min)
                            P.act(S_, G, AF.Sigmoid, scale=1.702)
                            P.ts("dve", U, psU, BGU[:, e, 4 + mt:5 + mt], ALU.add, 7.0, ALU.min)
                            P.ts("pool", U, U, -7.0, ALU.max, 1.0, ALU.add)
                            P.tt("pool", G, G, S_, ALU.mult)
                            P.tt("pool", U, U, CBE[c], ALU.mult)
                            P.tt("dve", A[c][mt], G, U, ALU.mult)
                for c in range(2):
                    for f in range(KT):
                        ps = P.ps(4 + nd % 2)
                        nd += 1
                        for k in range(4):
                            P.mm(ps, WDN[:, k, f * 128:(f + 1) * 128], A[c][k], start=(k == 0), stop=(k == 3))
                        P.tt("dve", ACC[f][c], ACC[f][c], ps, ALU.add)
            for c in range(2):
                ct = cts[c]
                s = ct // self.CPS
                V = []
                for f in range(KT):
                    xs = XS[f % 2][:, c * CT:(c + 1) * CT] if False else None
                    xt = CBE[0] if False else None
                    V.append(ACC[f][c])
                xbuf = [Gb[0], Gb[1], Ub[0], Ub[1]]
                for f in range(KT):
                    xb = xbuf[f % 4]
                    P.dma("sp", xb, self.dv(self.X1[f * 128:(f + 1) * 128, ct * CT:(ct + 1) * CT], "X1", ct))
                    P.stt("dve", V[f], V[f], self.mod(li, 5, f, s), xb, ALU.mult, ALU.add)
                self.resid_ln(li, 2, ct, V, dst, dname)
            P.release(m0)

    def rg_slab_tiles(self, j):
        b0, b1 = (128 * j) // 160, (128 * j + 127) // 160
        i0, i1 = (160 * b0) // 128, (160 * b1 + 159) // 128
        return list(range(i0, i1 + 1))

    def rg_layer(self, li, j, xsrc, xname):
        P = self.P
        m0 = P.mark()
        VEC = P.sb(f"rgvec{li}", [128, 9, RGT], F32)
        P.dma("sp", VEC, DV(self.rg_vec[:, j], "rg_vec"))
        cfe = VEC[:, 8, :]
        P.act(cfe, VEC[:, 7, :], AF.Exp, scale=-1.0)
        P.act(cfe, cfe, AF.Ln, bias=1.0)
        P.ts("dve", cfe, cfe, -8.0, ALU.mult)
        HST = P.sb(f"rghst{li}", [128, RGT], F32)
        HALO = P.sb(f"rghalo{li}", [128, RGT, 3], F32)
        X = [P.alloc([CT], F32) for _ in range(KT)]
        H = [P.alloc([CT], BF16) for _ in range(KT)]
        WB = [P.alloc([RGT, CT], BF16) for _ in range(2)]
        GATE = [P.alloc([CT], BF16) for _ in range(RGT)]
        XRB = [P.alloc([CT], BF16) for _ in range(RGT)]
        XRF = [P.alloc([CT], F32) for _ in range(2)]
        RECB = [P.alloc([CT + 4], F32) for _ in range(2)]
        SL = [P.alloc([2, 4, 128], BF16) for _ in range(2)]
        Rb = [P.alloc([CT], F32) for _ in range(2)]
        Ib = [P.alloc([CT], F32) for _ in range(2)]
        Ab = [P.alloc([CT], F32) for _ in range(2)]
        Tb = [P.alloc([CT], F32) for _ in range(2)]
        HS = [P.alloc([CT], F32) for _ in range(2)]
        nw = 0
        npz = 0
        for ct in range(self.NCT):
            s = ct // self.CPS
            if ct % self.CPS == 0:
                P.memset("pool", HST, 0.0)
                P.memset("pool", HALO, 0.0)
            self.load_x_tiles(xsrc, xname, ct, X)
            self.modulate(li, 1, ct, X, H)
            def body_in(nb, w):
                nonlocal npz
                for mi in range(4):
                    ft = nb * 4 + mi
                    ps = P.ps(npz % 4)
                    npz += 1
                    for k in range(KT):
                        P.mm(ps, w[:, k, mi * 128:(mi + 1) * 128], H[k], start=(k == 0), stop=(k == KT - 1))
                    if ft < RGT:
                        P.act(GATE[ft], ps, AF.Gelu_apprx_tanh)
                    else:
                        jj = ft - RGT
                        rb = RECB[jj % 2]
                        xr = XRF[jj % 2]
                        P.copy("act", rb[:, 3:3 + CT], ps)
                        P.copy("pool", rb[:, 0:3], HALO[:, jj, :])
                        P.copy("pool", HALO[:, jj, :], rb[:, CT:CT + 3])
                        P.act(xr, rb[:, 3:3 + CT], AF.Identity, bias=VEC[:, 4, jj:jj + 1], scale=VEC[:, 3, jj:jj + 1])
                        for d in (1, 2, 3):
                            P.stt("dve", xr, rb[:, 3 - d:3 - d + CT], VEC[:, 3 - d, jj:jj + 1], xr, ALU.mult, ALU.add)
                        P.copy("act", XRB[jj], xr)
            self.stream(WB, [(self.rg_w_in[j][:, nb * CT:(nb + 1) * CT], KT, "rg_w_in") for nb in range(10)], body_in)
            P.dma("pool", SL[0], DV(self.rg_slabs[j, 0], "rg_slabs"))
            for jt in range(RGT):
                tiles = self.rg_slab_tiles(jt)
                sl = SL[jt % 2]
                if jt + 1 < RGT:
                    P.dma("pool", SL[(jt + 1) % 2], DV(self.rg_slabs[j, jt + 1], "rg_slabs"))
                psR, psI = P.ps(4 + 2 * (jt % 2)), P.ps(5 + 2 * (jt % 2))
                for gi, ps in ((0, psR), (1, psI)):
                    for si, it in enumerate(tiles):
                        P.mm(ps, sl[:, gi, si, :], XRB[it], start=(si == 0), stop=(si == len(tiles) - 1))
                R, I, A_, T, hs = Rb[jt % 2], Ib[jt % 2], Ab[jt % 2], Tb[jt % 2], HS[jt % 2]
                P.act(R, psR, AF.Sigmoid, bias=VEC[:, 5, jt:jt + 1])
                P.act(I, psI, AF.Sigmoid, bias=VEC[:, 6, jt:jt + 1])
                P.act(A_, R, AF.Exp, scale=VEC[:, 8, jt:jt + 1])
                P.tt("pool", T, A_, A_, ALU.mult)
                P.ts("pool", T, T, -1.0, ALU.mult, 1.0, ALU.add)
                P.act(T, T, AF.Sqrt)
                P.tt("pool", I, I, XRB[jt], ALU.mult)
                P.tt("pool", T, T, I, ALU.mult)
                P.scan(hs, A_, T, HST[:, jt:jt + 1], ALU.mult, ALU.add)
                P.copy("act", HST[:, jt:jt + 1], hs[:, CT - 1:CT])
                P.tt("dve", GATE[jt], GATE[jt], hs, ALU.mult)
            def body_out(nb, w):
                nonlocal npz
                for mi in range(4):
                    ft = nb * 4 + mi
                    ps = P.ps(npz % 4)
                    npz += 1
                    for k in range(RGT):
                        P.mm(ps, w[:, k, mi * 128:(mi + 1) * 128], GATE[k], start=(k == 0), stop=(k == RGT - 1))
                    P.stt("dve", X[ft], ps, self.mod(li, 2, ft, s), X[ft], ALU.mult, ALU.add)
            self.stream(WB, [(self.rg_w_out[j][:, nb * CT:(nb + 1) * CT], RGT, "rg_w_out") for nb in range(4)], body_out)
            self.resid_ln(li, 1, ct, X, self.X1, "X1")
        P.release(m0)

    def bcast_rows(self, dst, src_ap, name):
        n = src_ap.shape[-1]
        self.P.dma("sp", dst, DV(src_ap.broadcast_to([128, n]), name))

    def sgu_layer(self, li, j, xsrc, xname):
        P = self.P
        m0 = P.mark()
        VP = P.sb(f"sgvp{li}", [128, 2, KT], F32)
        P.dma("sp", VP, DV(self.sg_vecp[:, j], "sg_vecp"))
        BIASV = P.alloc([D], F32)
        LNG = P.alloc([D], F32)
        LNB = P.alloc([D], F32)
        BSP = P.alloc([8, 128], F32)
        self.bcast_rows(BIASV, self.sg_rows[j, 0:1, :], "sg_rows")
        self.bcast_rows(LNG, self.sg_rows[j, 1:2, :], "sg_rows")
        self.bcast_rows(LNB, self.sg_rows[j, 2:3, :], "sg_rows")
        self.bcast_rows(BSP.re("p g i -> p (g i)"), self.sg_rows[j, 3:4, 0:1024], "sg_rows")
        WSTf = P.alloc([8, 128], F32)
        WSTb = P.alloc([8, 128], BF16)
        P.dma("sp", WSTf, DV(self.sg_w_spT[j], "sg_w_spT"))
        P.memset("pool", WSTf[64:128, :, 0:64], 0.0)
        P.copy("pool", WSTb, WSTf)
        H = [P.alloc([CT], BF16) for _ in range(KT)]
        WB = [P.alloc([KT, CT], BF16) for _ in range(2)]
        U = [P.alloc([CT], BF16) for _ in range(KT)]
        VT = [P.alloc([D], F32) for _ in range(4)]
        X = [P.alloc([CT], F32, at=VT[0].off + f * 2048) for f in range(KT)]
        VN = [P.alloc([D], BF16) for _ in range(4)]
        XS = [P.alloc([CT], F32) for _ in range(2)]
        T = [P.alloc([CT], F32) for _ in range(2)]
        ST = P.alloc([4, 6], F32)
        MV = P.alloc([4], F32)
        nw = 0
        npz = 0
        for ct in range(self.NCT):
            s = ct // self.CPS
            for k in range(KT):
                xs = XS[k % 2]
                P.dma("sp", xs, self.dv(xsrc[k * 128:(k + 1) * 128, ct * CT:(ct + 1) * CT], xname, ct))
                P.act(H[k], xs, AF.Identity, bias=self.mod(li, 0, k, s), scale=self.mod(li, 1, k, s))
            def body_uv(nb, w):
                nonlocal npz
                if nb < 4:
                    for mi in range(4):
                        ft = nb * 4 + mi
                        ps = P.ps(npz % 4)
                        npz += 1
                        for k in range(KT):
                            P.mm(ps, w[:, k, mi * 128:(mi + 1) * 128], H[k], start=(k == 0), stop=(k == KT - 1))
                        P.act(U[ft], ps, AF.Gelu_apprx_tanh, bias=VP[:, 0, ft:ft + 1])
                    return
                nb -= 4
                for tt in range(4):
                    ps = P.ps(npz % 4)
                    npz += 1
                    for k in range(KT):
                        P.mm(ps, H[k][:, tt * 128:(tt + 1) * 128], w[:, k, :], start=(k == 0), stop=(k == KT - 1))
                    vt = VT[tt][:, nb * CT:(nb + 1) * CT]
                    P.tt("dve", vt, ps, BIASV[:, nb * CT:(nb + 1) * CT], ALU.add)
                    P.act(vt, vt, AF.Gelu_apprx_tanh)
            self.stream(WB, [(self.sg_w_in[j][:, nb * CT:(nb + 1) * CT], KT, "sg_w_in") for nb in range(8)], body_uv)
            for tt in range(4):
                P.reduce_add(MV[:, 0:1], VT[tt])
                P.act(VN[tt], VT[tt], AF.Square, accum=MV[:, 1:2])
                P.ts("dve", MV[:, 0:1], MV[:, 0:1], 1.0 / D, ALU.mult)
                P.tt("dve", MV[:, 3:4], MV[:, 0:1], MV[:, 0:1], ALU.mult)
                P.stt("dve", MV[:, 1:2], MV[:, 1:2], 1.0 / D, MV[:, 3:4], ALU.mult, ALU.subtract)
                P.act(MV[:, 2:3], MV[:, 1:2], AF.Sqrt, bias=self.epsc[:, 1:2])
                P.recip(MV[:, 2:3], MV[:, 2:3])
                P.ts("dve", VT[tt], VT[tt], MV[:, 0:1], ALU.subtract, MV[:, 2:3], ALU.mult)
                P.tt("pool", VT[tt], VT[tt], LNG, ALU.mult)
                P.tt("pool", VN[tt], VT[tt], LNB, ALU.add)
            for c in range(KT):
                g = c // 2
                ps = P.ps(4 + c % 2)
                for tt in range(4):
                    P.mm(ps[:, tt * 128:(tt + 1) * 128], VN[tt][:, c * 128:(c + 1) * 128], WSTb[:, g, :])
                t = T[c % 2]
                bsp = BSP[:, g, :]
                P.tt("dve", t.re("p (a i) -> p a i", i=128), ps.re("p (a i) -> p a i", i=128),
                     View(bsp.ap.unsqueeze(1).broadcast_to([128, 4, 128]), bsp.keys), ALU.add)
                P.tt("pool", U[c], U[c], t, ALU.mult)
            for f in range(KT):
                P.dma("sp", X[f], self.dv(xsrc[f * 128:(f + 1) * 128, ct * CT:(ct + 1) * CT], xname, ct))
            def body_o(nb, w):
                nonlocal npz
                for mi in range(4):
                    ft = nb * 4 + mi
                    ps = P.ps(npz % 4)
                    npz += 1
                    for k in range(KT):
                        P.mm(ps, w[:, k, mi * 128:(mi + 1) * 128], U[k], start=(k == 0), stop=(k == KT - 1))
                    t = T[ft % 2]
                    P.act(t, ps, AF.Identity, bias=VP[:, 1, ft:ft + 1])
                    P.stt("dve", X[ft], t, self.mod(li, 2, ft, s), X[ft], ALU.mult, ALU.add)
            self.stream(WB, [(self.sg_w_out[j][:, nb * CT:(nb + 1) * CT], KT, "sg_w_out") for nb in range(4)], body_o)
            self.resid_ln(li, 1, ct, X, self.X1, "X1")
        P.release(m0)

    def ml_layer(self, li, j, xsrc, xname):
        P = self.P
        S, NT, NS = self.S, self.CPS, self.NSEQ
        NTT = S // 128
        if not hasattr(self, "ml_scr"):
            self.ml_scr = dict(
                QT=P.dram("mlQT", [D, self.NTOK], BF16), KTs=P.dram("mlKT", [D, self.NTOK], BF16),
                VTK=P.dram("mlVTK", [self.NTOK, D], BF16), XC=P.dram("mlXC", [D, self.NTOK], F32),
                SZ=P.dram("mlSZ", [D, self.NTOK], F32))
            self.ONES256 = P.sb("ones256", [128, 128], F32)
            P.memset("pool", self.ONES256, 1.0 / 256)
            self.ONESB = P.sb("onesb", [128, 128], BF16)
            P.memset("pool", self.ONESB, 1.0)
            self.SELH = P.sb("selh", [8, 8, 128], F32)
            P.copy("pool", self.SELH, View(self.ident.ap[0:8, 0:8].unsqueeze(2).broadcast_to([8, 8, 128]), self.ident.keys))
        scr = self.ml_scr
        m0 = P.mark()
        VP = P.sb(f"mlvp{li}", [128, 7, KT], F32)
        P.dma("sp", VP, DV(self.ml_vecp[:, j], "ml_vecp"))
        BG = P.sb(f"mlbg{li}", [8, 3], F32)
        P.dma("sp", BG[:, 0:2], DV(self.ml_b_gates[:, j], "ml_b_gates"))
        P.ts("dve", BG[:, 2:3], BG[:, 1:2], -1.0, ALU.mult)
        HALO = P.sb(f"mlhalo{li}", [128, KT, 3], F32)
        BD = [P.alloc([KT, 128], BF16) for _ in range(3)]
        for qi in range(3):
            self.P.dma("pool", BD[qi], DV(self.ml_bd[j, qi], "ml_bd"))
        WG = P.alloc([48, 16], BF16)
        P.dma("pool", WG, DV(self.ml_w_gates[j], "ml_w_gates"))
        MASK = P.alloc([4, 512], F32)
        P.dma("sp", MASK, DV(self.maskneg_d, "maskneg"))
        GI = P.alloc([S], F32)
        LF = P.alloc([S], F32)
        BT = P.alloc([S], F32)
        CTK = P.alloc([NTT, 8], F32)
        m1 = P.mark()
        for s in range(NS):
            P.release(m1)
            H = [P.alloc([CT], BF16) for _ in range(KT)]
            WB = [P.alloc([KT, CT], BF16) for _ in range(2)]
            XCB = [P.alloc([CT], BF16) for _ in range(KT)]
            XMB = [P.alloc([CT], BF16) for _ in range(KT)]
            XS = [P.alloc([CT], F32) for _ in range(2)]
            RECB = [P.alloc([CT + 4], F32) for _ in range(2)]
            XCF = [P.alloc([CT], F32) for _ in range(2)]
            QB = [P.alloc([CT], BF16) for _ in range(3)]
            VST = [P.alloc([4, 128], BF16) for _ in range(2)]
            TG_ = [P.alloc([CT], F32) for _ in range(2)]
            nw = 0
            npz = 0
            P.memset("pool", HALO, 0.0)
            for c in range(NT):
                ct = s * NT + c
                for k in range(KT):
                    xs = XS[k % 2]
                    P.dma("sp", xs, self.dv(xsrc[k * 128:(k + 1) * 128, ct * CT:(ct + 1) * CT], xname, ct))
                    P.act(H[k], xs, AF.Identity, bias=self.mod(li, 0, k, s), scale=self.mod(li, 1, k, s))
                def body_in(nb, w):
                    nonlocal npz
                    for mi in range(4):
                        ft = nb * 4 + mi
                        ps = P.ps(npz % 4)
                        npz += 1
                        for k in range(KT):
                            P.mm(ps, w[:, k, mi * 128:(mi + 1) * 128], H[k], start=(k == 0), stop=(k == KT - 1))
                        if ft < KT:
                            rb, xc = RECB[ft % 2], XCF[ft % 2]
                            P.copy("act", rb[:, 3:3 + CT], ps)
                            P.copy("pool", rb[:, 0:3], HALO[:, ft, :])
                            P.copy("pool", HALO[:, ft, :], rb[:, CT:CT + 3])
                            P.copy("pool", XMB[ft], rb[:, 3:3 + CT])
                            P.act(xc, rb[:, 3:3 + CT], AF.Identity, bias=VP[:, 4, ft:ft + 1], scale=VP[:, 3, ft:ft + 1])
                            for d in (1, 2, 3):
                                P.stt("dve", xc, rb[:, 3 - d:3 - d + CT], VP[:, 3 - d, ft:ft + 1], xc, ALU.mult, ALU.add)
                            P.act(xc, xc, AF.Silu)
                            P.copy("pool", XCB[ft], xc)
                            P.dma("sp", self.dv(scr["XC"][ft * 128:(ft + 1) * 128, ct * CT:(ct + 1) * CT], "mlXC", ct), xc)
                        else:
                            f2 = ft - KT
                            t = TG_[f2 % 2]
                            P.act(t, ps, AF.Sigmoid)
                            P.dma("sp", self.dv(scr["SZ"][f2 * 128:(f2 + 1) * 128, ct * CT:(ct + 1) * CT], "mlSZ", ct), t)
                self.stream(WB, [(self.ml_w_in[j][:, nb * CT:(nb + 1) * CT], KT, "ml_w_in") for nb in range(8)], body_in)
                psGI, psGF = P.ps(6), P.ps(7)
                ng = 0
                for ft in range(KT):
                    for qi, src, dname in ((0, XCB, "QT"), (1, XCB, "KTs"), (2, XMB, None)):
                        ps = P.ps(npz % 4)
                        npz += 1
                        P.mm(ps, BD[qi][:, ft, :], src[ft])
                        qb = QB[qi]
                        P.copy("act" if qi != 1 else "dve", qb, ps)
                        if dname is not None:
                            P.dma("sp", self.dv(scr[dname][ft * 128:(ft + 1) * 128, ct * CT:(ct + 1) * CT], "ml" + dname, ct), qb)
                        P.mm(psGI[0:8], WG[:, qi * KT + ft, 0:8], qb, start=(ng == 0), stop=(ng == 47))
                        P.mm(psGF[0:8], WG[:, qi * KT + ft, 8:16], qb, start=(ng == 0), stop=(ng == 47))
                        ng += 1
                    ps = P.ps(npz % 4)
                    npz += 1
                    for tt in range(4):
                        P.mm(ps[:, tt * 128:(tt + 1) * 128], XMB[ft][:, tt * 128:(tt + 1) * 128], BD[2][:, ft, :])
                    vst = VST[ft % 2]
                    P.copy("dve", vst, ps.re("p (a f) -> p a f", f=128))
                    P.dma("sp", self.dv(scr["VTK"][ct * CT:(ct + 1) * CT, ft * 128:(ft + 1) * 128].rearrange("(t p) f -> p t f", p=128), "mlVTK", ct), vst)
                cs = slice(c * CT, (c + 1) * CT)
                P.act(GI[0:8, cs], psGI[0:8], AF.Identity, bias=BG[:, 0:1])
                P.act(LF[0:8, cs], psGF[0:8], AF.Exp, bias=BG[:, 2:3], scale=-1.0)
                P.act(LF[0:8, cs], LF[0:8, cs], AF.Ln, bias=1.0)
            P.release(m1)
            P.ts("dve", LF[0:8], LF[0:8], -1.0, ALU.mult)
            P.scan(BT[0:8], self.onec[0:8, 0:1].bc([8, S]), LF[0:8], 0.0, ALU.mult, ALU.add)
            P.tt("dve", GI[0:8], GI[0:8], BT[0:8], ALU.subtract)
            pt = P.ps(5)
            for t in range(NTT):
                P.tr(pt[:, t * 8:(t + 1) * 8], GI[0:8, t * 128:(t + 1) * 128], self.ident[0:8, 0:8])
            P.copy("dve", CTK, pt[:, 0:NTT * 8].re("p (t e) -> p t e", e=8))
            GOUT = [P.alloc([S], BF16) for _ in range(KT)]
            m2 = P.mark()
            QH = [[P.alloc([S], BF16) for _ in range(2)] for _ in range(1)]
            KH = [[P.alloc([S], BF16) for _ in range(2)] for _ in range(1)]
            VH = [P.alloc([NTT, 256], BF16) for _ in range(1)]
            BBC = P.alloc([CT], F32)
            DM = [P.alloc([CT], F32) for _ in range(2)]
            PM = [P.alloc([CT], BF16) for _ in range(2)]
            RD = P.alloc([CT], F32)
            HS = [P.alloc([CT], F32) for _ in range(2)]
            SQ = [P.alloc([CT], F32) for _ in range(2)]
            XCt = [P.alloc([CT], F32) for _ in range(2)]
            SZt = [P.alloc([CT], F32) for _ in range(2)]
            MEAN = P.alloc([CT], F32)
            RSTD = P.alloc([CT], F32)
            t0, t1 = s * S, (s + 1) * S
            seqkeys = lambda nm: [("d", nm, s * NT + c) for c in range(NT)]
            nps = 0
            for h in range(8):
                qh, kh, vh = QH[0], KH[0], VH[0]
                for d in range(2):
                    f = 2 * h + d
                    P.dma("sp", qh[d], View(scr["QT"][f * 128:(f + 1) * 128, t0:t1], seqkeys("mlQT")))
                    P.dma("sp", kh[d], View(scr["KTs"][f * 128:(f + 1) * 128, t0:t1], seqkeys("mlKTs")))
                P.dma("sp", vh, View(scr["VTK"][t0:t1, h * 256:(h + 1) * 256].rearrange("(t p) f -> p t f", p=128), seqkeys("mlVTK")))
                for tq in range(NT):
                    ct = s * NT + tq
                    ts_ = slice(tq * CT, (tq + 1) * CT)
                    pb = P.ps(5)
                    P.mm(pb, self.SELH[0:8, h, :], BT[0:8, ts_])
                    P.copy("act", BBC, pb)
                    pn0, pn1, pd = P.ps(2), P.ps(3), P.ps(4)
                    ns = 4 * (tq + 1)
                    for si in range(ns):
                        pS = P.ps(nps % 2)
                        dm, pm = DM[nps % 2], PM[nps % 2]
                        nps += 1
                        ss = slice(si * 128, (si + 1) * 128)
                        P.mm(pS, kh[0][:, ss], qh[0][:, ts_], start=True, stop=False)
                        P.mm(pS, kh[1][:, ss], qh[1][:, ts_], start=False, stop=True)
                        cst = CTK[:, si, h:h + 1]
                        if si >= 4 * tq:
                            P.stt("dve", dm, BBC, cst, MASK[:, si - 4 * tq, :], ALU.add, ALU.add)
                            P.act(dm, dm, AF.Exp)
                        else:
                            P.act(dm, BBC, AF.Exp, bias=cst)
                        P.stt("dve", pm, pS, 1.0 / 16.0, dm, ALU.mult, ALU.mult)
                        P.mm(pn0, vh[:, si, 0:128], pm, start=(si == 0), stop=(si == ns - 1))
                        P.mm(pn1, vh[:, si, 128:256], pm, start=(si == 0), stop=(si == ns - 1))
                        P.mm(pd, self.ONESB, pm, start=(si == 0), stop=(si == ns - 1))
                    P.act(RD, pd, AF.Abs)
                    P.ts("dve", RD, RD, 1.0, ALU.max)
                    P.recip(RD, RD)
                    P.tt("dve", HS[0], pn0, RD, ALU.mult)
                    P.tt("dve", HS[1], pn1, RD, ALU.mult)
                    pm_, pe_ = P.ps(6), P.ps(7)
                    for d in range(2):
                        P.mm(pm_, self.ONES256, HS[d], start=(d == 0), stop=(d == 1))
                    for d in range(2):
                        P.act(SQ[d], HS[d], AF.Square)
                        P.mm(pe_, self.ONES256, SQ[d], start=(d == 0), stop=(d == 1))
                    P.copy("act", MEAN, pm_)
                    P.tt("pool", SQ[0], MEAN, MEAN, ALU.mult)
                    P.tt("dve", RSTD, pe_, SQ[0], ALU.subtract)
                    P.act(RSTD, RSTD, AF.Sqrt, bias=self.epsc[:, 1:2])
                    P.recip(RSTD, RSTD)
                    for d in range(2):
                        f = 2 * h + d
                        P.dma("sp", XCt[d], self.dv(scr["XC"][f * 128:(f + 1) * 128, ct * CT:(ct + 1) * CT], "mlXC", ct))
                        P.dma("sp", SZt[d], self.dv(scr["SZ"][f * 128:(f + 1) * 128, ct * CT:(ct + 1) * CT], "mlSZ", ct))
                        P.tt("dve", HS[d], HS[d], MEAN, ALU.subtract)
                        P.tt("pool", HS[d], HS[d], RSTD, ALU.mult)
                        P.ts("pool", HS[d], HS[d], VP[:, 6, f:f + 1], ALU.mult)
                        P.stt("dve", HS[d], XCt[d], VP[:, 5, f:f + 1], HS[d], ALU.mult, ALU.add)
                        P.tt("pool", GOUT[f][:, ts_], HS[d], SZt[d], ALU.mult)
            P.release(m2)
            X = [P.alloc([CT], F32) for _ in range(KT)]
            WB = [P.alloc([KT, CT], BF16) for _ in range(2)]
            for c in range(NT):
                ct = s * NT + c
                ts_ = slice(c * CT, (c + 1) * CT)
                self.load_x_tiles(xsrc, xname, ct, X)
                def body_o(nb, w):
                    nonlocal npz
                    for mi in range(4):
                        ft = nb * 4 + mi
                        ps = P.ps(npz % 4)
                        npz += 1
                        for k in range(KT):
                            P.mm(ps, w[:, k, mi * 128:(mi + 1) * 128], GOUT[k][:, ts_], start=(k == 0), stop=(k == KT - 1))
                        P.stt("dve", X[ft], ps, self.mod(li, 2, ft, s), X[ft], ALU.mult, ALU.add)
                self.stream(WB, [(self.ml_w_out[j][:, nb * CT:(nb + 1) * CT], KT, "ml_w_out") for nb in range(4)], body_o)
                self.resid_ln(li, 1, ct, X, self.X1, "X1")
        P.release(m0)


def _pp(v):
    v = np.asarray(v, np.float32)
    sh = v.shape
    v = v.reshape(sh[:-1] + (sh[-1] // 128, 128))
    return np.ascontiguousarray(np.moveaxis(v, -1, 0))


def rg_slab_tiles(j):
    b0, b1 = (128 * j) // 160, (128 * j + 127) // 160
    i0, i1 = (160 * b0) // 128, (160 * b1 + 159) // 128
    return list(range(i0, i1 + 1))


class _Sel:
    def __init__(self, inp, sel):
        self.inp, self.sel = inp, sel

    def __getitem__(self, name):
        return _SelArr(self.inp[name], self.sel[name])


class _SelArr:
    def __init__(self, a, idx):
        self.a, self.idx = a, idx

    def __getitem__(self, key):
        if isinstance(key, (int, np.integer)):
            return np.asarray(self.a[self.idx[key]], np.float32)
        g = np.stack([np.asarray(self.a[i], np.float32) for i in self.idx], axis=0)
        if isinstance(key, tuple):
            return g[(slice(None),) + key[1:]]
        return g


def prep_shared(inp_raw, layers, li_ids=None):
    L = len(layers)
    li_ids = list(range(L)) if li_ids is None else list(li_ids)
    jsel = {k: [j for kk, j in layers if kk == k] or [0] for k in (0, 1, 2)}
    sel = {}
    for name in inp_raw:
        if name.startswith("rg_"):
            sel[name] = jsel[0]
        elif name.startswith("sg_"):
            sel[name] = jsel[1]
        elif name.startswith("ml_"):
            sel[name] = jsel[2]
        else:
            sel[name] = li_ids
    inp = _Sel(inp_raw, sel)
    out = {}
    f = lambda a: np.ascontiguousarray(np.asarray(a, np.float32))
    out["ada_w"] = f(inp["ada_w"][:L])
    out["ada_bT"] = _pp(inp["ada_b"][:L])
    lnp = np.stack([inp["ln1_g"][:L], inp["ln1_b"][:L], inp["ln2_g"][:L], inp["ln2_b"][:L]], axis=1)
    out["lnp"] = _pp(lnp)
    out["ident"] = np.eye(128, dtype=np.float32)
    mk = np.zeros((128, 4, 512), np.float32)
    for o in range(4):
        sidx = o * 128 + np.arange(128)[:, None]
        tidx = np.arange(512)[None, :]
        mk[:, o, :] = np.where(sidx <= tidx, 0.0, -30000.0)
    out["maskneg"] = mk
    nA = max(1, sum(1 for k, _ in layers if k == 0))
    nB = max(1, sum(1 for k, _ in layers if k == 1))
    nC = max(1, sum(1 for k, _ in layers if k == 2))
    out["rg_w_in"] = f(inp["rg_w_in"][:nA])
    vec = np.concatenate([inp["rg_conv_w"][:nA], inp["rg_conv_b"][:nA, None], inp["rg_b_rgate"][:nA, None],
                          inp["rg_b_igate"][:nA, None], inp["rg_lam"][:nA, None], np.zeros_like(inp["rg_lam"][:nA, None])], axis=1)
    out["rg_vec"] = _pp(vec)
    slabs = np.zeros((nA, RGT, 128, 2, 4, 128), np.float32)
    for a in range(nA):
        for gi, wname in enumerate(("rg_w_rgate", "rg_w_igate")):
            w = np.asarray(inp[wname][a], np.float32)
            for jt in range(RGT):
                for si, it in enumerate(rg_slab_tiles(jt)):
                    for n in range(16):
                        r0, r1 = max(it * 128, n * 160), min(it * 128 + 128, n * 160 + 160)
                        c0, c1 = max(jt * 128, n * 160), min(jt * 128 + 128, n * 160 + 160)
                        if r0 < r1 and c0 < c1:
                            slabs[a, jt, r0 - it * 128:r1 - it * 128, gi, si, c0 - jt * 128:c1 - jt * 128] = \
                                w[n, r0 - n * 160:r1 - n * 160, c0 - n * 160:c1 - n * 160]
    out["rg_slabs"] = slabs
    out["rg_w_out"] = f(inp["rg_w_out"][:nA])
    out["sg_w_in"] = f(inp["sg_w_in"][:nB])
    out["sg_vecp"] = _pp(np.stack([inp["sg_b_in"][:nB, :D], inp["sg_b_out"][:nB]], axis=1))
    bsp = np.zeros((nB, D), np.float32)
    bsp[:, :1024] = np.asarray(inp["sg_b_sp"][:nB]).reshape(nB, 1024)
    out["sg_rows"] = f(np.stack([inp["sg_b_in"][:nB, D:], inp["sg_ln_g"][:nB], inp["sg_ln_b"][:nB], bsp], axis=1))
    out["sg_w_spT"] = f(np.transpose(np.asarray(inp["sg_w_sp"][:nB]), (0, 3, 1, 2)))
    out["sg_w_out"] = f(inp["sg_w_out"][:nB])
    out["ml_w_in"] = f(inp["ml_w_in"][:nC])
    out["ml_vecp"] = _pp(np.concatenate([inp["ml_conv_w"][:nC], inp["ml_conv_b"][:nC, None], inp["ml_skip"][:nC, None],
                                         inp["ml_norm_g"][:nC, None]], axis=1))
    bd = np.zeros((nC, 3, 128, KT, 128), np.float32)
    for a in range(nC):
        for qi, wname in enumerate(("ml_w_q", "ml_w_k", "ml_w_v")):
            w = np.asarray(inp[wname][a], np.float32)
            for t in range(KT):
                for b in range(32):
                    bd[a, qi, b * 4:b * 4 + 4, t, b * 4:b * 4 + 4] = w[t * 32 + b]
    out["ml_bd"] = bd
    wg = np.asarray(inp["ml_w_gates"][:nC], np.float32).reshape(nC, 48, 128, 16)
    out["ml_w_gates"] = f(np.transpose(wg, (0, 2, 1, 3)))
    out["ml_b_gates"] = f(np.transpose(np.asarray(inp["ml_b_gates"][:nC]).reshape(nC, 2, 8), (2, 0, 1)))
    out["ml_w_out"] = f(inp["ml_w_out"][:nC])
    wr = np.asarray(inp["moe_w_router"][:L], np.float32).reshape(L, KT, 128, NE)
    out["moe_w_router"] = f(np.transpose(wr, (0, 2, 1, 3)))
    out["moe_b_router"] = f(np.asarray(inp["moe_b_router"][:L]).reshape(L, NE, 1))
    out["moe_w_gate_up"] = f(inp["moe_w_gate_up"][:L])
    out["moe_b_gu"] = _pp(inp["moe_b_gate_up"][:L])
    out["moe_w_down"] = f(inp["moe_w_down"][:L])
    out["moe_b_down"] = f(inp["moe_b_down"][:L])
    return out


def prep_core(x, c, b0, nseq):
    xs = np.asarray(x[b0:b0 + nseq], np.float32)
    S = xs.shape[1]
    xT = np.ascontiguousarray(xs.reshape(nseq * S, D).T)
    cT = _pp(np.asarray(c[b0:b0 + nseq], np.float32))
    cT = np.ascontiguousarray(np.transpose(cT, (0, 2, 1)))
    return {"xT": xT, "cT": cT}


_CACHE = {}


def run(inp, layers, nseq, ncores, trace=False, li_ids=None):
    x = np.asarray(inp["x"])
    B, S, _ = x.shape
    cnt = {0: 0, 1: 0, 2: 0}
    local = []
    for k, j in layers:
        local.append((k, cnt[k]))
        cnt[k] += 1
    key = (S, nseq, tuple(local))
    if key not in _CACHE:
        _CACHE[key] = MK(NSEQ=nseq, S=S, layers=local)
    mk = _CACHE[key]
    shared = prep_shared(inp, layers, li_ids)
    in_maps = []
    for r in range(ncores):
        d = dict(shared)
        d.update(prep_core(x, inp["c"], r * nseq, nseq))
        for n, (shape, _) in mk.inputs.items():
            assert tuple(d[n].shape) == shape, (n, d[n].shape, shape)
        in_maps.append({n: d[n] for n in mk.inputs})
    res = run_bass_kernel_spmd(mk.nc, in_maps, core_ids=list(range(ncores)), trace=trace)
    outs = []
    for r in range(ncores):
        oT = np.asarray(res.results[r]["outT"])
        outs.append(oT.T.reshape(nseq, S, D))
    return np.concatenate(outs, axis=0), res


LAUNCHES = [([(0, 0), (1, 0)], [0, 1]), ([(2, 0), (0, 1)], [2, 3])]


def kernel(**inputs):
    inp = dict(inputs)
    out = None
    for layers, li_ids in LAUNCHES:
        out, _ = run(inp, layers, nseq=2, ncores=8, li_ids=li_ids)
        inp["x"] = out
    return out.astype(np.float32)
```

```python
import numpy as np
from contextlib import ExitStack
import concourse.bass as bass
import concourse.mybir as mybir
from concourse.bass_utils import run_bass_kernel_spmd

F32 = mybir.dt.float32
BF16 = mybir.dt.bfloat16
AF = mybir.ActivationFunctionType
ALU = mybir.AluOpType

D = 2048
KT = 16
CT = 512
RGW = 2560
RGT = 20
NE = 32
DE = 512
ALPHA = 8 ** 0.25
LN_EPS = 1e-5
SAME_ENG_SYNC = True


def dsize(dt):
    return 2 if dt == BF16 else 4


class View:
    __slots__ = ("ap", "keys", "off")

    def __init__(self, ap, keys, off=None):
        self.ap = ap
        self.keys = keys
        self.off = off

    def __getitem__(self, idx):
        return View(self.ap[idx], self.keys)

    def re(self, pat, **kw):
        return View(self.ap.rearrange(pat, **kw), self.keys)

    def bc(self, shape):
        return View(self.ap.broadcast_to(shape), self.keys)


class Prog:
    ENGS = ("pe", "act", "dve", "pool", "sp")
    NDMA = {"sp": 12, "pool": 12}

    def __init__(self, nc, arena_kib=168):
        self.nc = nc
        self.es = ExitStack()
        self.streams = {e: [] for e in self.ENGS}
        self.cnt = {e: 0 for e in self.ENGS}
        self.clock = {e: {} for e in self.ENGS}
        self.dcnt = {}
        self.drr = {q: 0 for q in self.NDMA}
        self.lw = {}
        self.rd = {}
        self.arena_bytes = arena_kib * 1024
        self.arena = self.es.enter_context(nc.sbuf_tensor("arena", [128, self.arena_bytes // 4], F32))
        self.top = 0
        self.psb = [self.es.enter_context(nc.psum_tensor(f"psb{i}", [128, 512], F32)) for i in range(8)]
        self.ninst = 0

    def alloc(self, shape, dt, at=None):
        n = int(np.prod(shape)) * dsize(dt)
        nr = (n + 1023) // 1024 * 1024
        if at is None:
            off = self.top
            self.top += nr
            assert self.top <= self.arena_bytes, f"arena overflow {self.top}"
        else:
            off = at
            assert off % 1024 == 0 and off + nr <= self.arena_bytes
        ap = self.arena[:, off // 4:(off + n + 3) // 4]
        if dt != F32:
            ap = ap.bitcast(dt)
        if len(shape) == 2:
            ap = ap.rearrange("p (a b) -> p a b", a=shape[0])
        elif len(shape) == 3:
            ap = ap.rearrange("p (a b c) -> p a b c", a=shape[0], b=shape[1])
        return View(ap, [("a", p) for p in range(off // 1024, (off + nr) // 1024)], off)

    def mark(self):
        return self.top

    def release(self, m):
        self.top = m

    def sb(self, name, shape, dt):
        t = self.es.enter_context(self.nc.sbuf_tensor(name, list(shape), dt))
        return View(t[:], [("t", name)])

    def ps(self, b, dt=F32):
        return View(self.psb[b][:], [("ps", b)])

    def dram(self, name, shape, dt, kind="Internal"):
        t = self.nc.dram_tensor(name, list(shape), dt, kind=kind).ap()
        return t

    def _deps(self, reads, writes):
        deps = []
        for v in reads:
            for k in v.keys:
                e = self.lw.get(k)
                if e is not None:
                    deps.append(e)
        for v in writes:
            for k in v.keys:
                e = self.lw.get(k)
                if e is not None:
                    deps.append(e)
                r = self.rd.get(k)
                if r:
                    deps.extend(r)
        return deps

    def _waits(self, eng, deps):
        clk = self.clock[eng]
        need = {}
        for (sem, val, peng) in deps:
            if peng == eng and (eng == "pe" or not SAME_ENG_SYNC):
                continue
            if clk.get(sem, 0) >= val:
                continue
            if need.get(sem, 0) < val:
                need[sem] = val
        for sem, val in need.items():
            clk[sem] = val
        return list(need.items())

    def _record(self, ev, reads, writes):
        for v in writes:
            for k in v.keys:
                self.lw[k] = ev
                self.rd[k] = []
        for v in reads:
            for k in v.keys:
                lst = self.rd.setdefault(k, [])
                lst[:] = [x for x in lst if x[0] != ev[0]]
                lst.append(ev)

    def op(self, eng, fn, reads, writes):
        reads = [v for v in reads if isinstance(v, View)]
        waits = self._waits(eng, self._deps(reads, writes))
        self.cnt[eng] += 1
        ev = (eng, self.cnt[eng], eng)
        self.streams[eng].append((waits, fn, (eng, 1)))
        self._record(ev, reads, writes)
        self.ninst += 1
        return ev

    def dma(self, q, out, in_):
        i = self.drr[q]
        self.drr[q] = (i + 1) % self.NDMA[q]
        sem = f"d{q}{i}"
        c = self.dcnt.get(sem, 0)
        deps = self._deps([in_], [out])
        if c > 0:
            deps.append((sem, 16 * c, "dma"))
        waits = self._waits(q, deps)
        self.dcnt[sem] = c + 1
        ev = (sem, 16 * (c + 1), "dma")
        oap, iap = out.ap, in_.ap
        self.streams[q].append((waits, lambda e: e.dma_start(out=oap, in_=iap), (sem, 16)))
        self._record(ev, [in_], [out])
        self.ninst += 1
        return ev

    def wait_all(self, eng, evs):
        waits = self._waits(eng, evs)
        self.streams[eng].append((waits, None, None))

    def emit(self):
        nc = self.nc
        names = set(self.ENGS) | set(self.dcnt.keys())
        sems = {n: self.es.enter_context(nc.semaphore(n)) for n in sorted(names)}
        block = self.es.enter_context(nc.Block())

        def runner(en):
            def f(eng):
                for waits, fn, inc in self.streams[en]:
                    if fn is None:
                        for (s, v) in waits:
                            eng.wait_ge(sems[s], v)
                        continue
                    for (s, v) in waits[1:]:
                        eng.wait_ge(sems[s], v)
                    ins = fn(eng)
                    if waits:
                        ins._wait_ge(sems[waits[0][0]], waits[0][1])
                    ins.then_inc(sems[inc[0]], inc[1])
            return f
        block.tensor(runner("pe"))
        block.scalar(runner("act"))
        block.vector(runner("dve"))
        block.gpsimd(runner("pool"))
        block.sync(runner("sp"))
        self.es.close()

    @staticmethod
    def _a(x):
        return x.ap if isinstance(x, View) else x

    def mm(self, out, lhsT, rhs, start=True, stop=True):
        o, l, r = out.ap, lhsT.ap, rhs.ap
        return self.op("pe", lambda e: e.matmul(o, lhsT=l, rhs=r, start=start, stop=stop), [lhsT, rhs], [out])

    def tr(self, out, in_, ident):
        o, i, d = out.ap, in_.ap, ident.ap
        return self.op("pe", lambda e: e.transpose(o, i, d), [in_, ident], [out])

    def act(self, out, in_, func, bias=0.0, scale=1.0, accum=None):
        o, i, b, s = out.ap, in_.ap, self._a(bias), self._a(scale)
        if accum is not None:
            a = accum.ap
            return self.op("act", lambda e: e.activation(out=o, in_=i, func=func, bias=b, scale=s, accum_out=a), [in_, bias, scale], [out, accum])
        return self.op("act", lambda e: e.activation(out=o, in_=i, func=func, bias=b, scale=s), [in_, bias, scale], [out])

    def reduce_add(self, out, in_):
        o, i = out.ap, in_.ap
        return self.op("dve", lambda e: e.tensor_reduce(out=o, in_=i, axis=mybir.AxisListType.X, op=ALU.add), [in_], [out])

    def tt(self, eng, out, in0, in1, op):
        o, a, b = out.ap, in0.ap, in1.ap
        return self.op(eng, lambda e: e.tensor_tensor(out=o, in0=a, in1=b, op=op), [in0, in1], [out])

    def ts(self, eng, out, in0, s1, op0, s2=None, op1=None):
        o, a, x1, x2 = out.ap, in0.ap, self._a(s1), self._a(s2)
        if op1 is None:
            return self.op(eng, lambda e: e.tensor_scalar(out=o, in0=a, scalar1=x1, scalar2=None, op0=op0), [in0, s1], [out])
        return self.op(eng, lambda e: e.tensor_scalar(out=o, in0=a, scalar1=x1, scalar2=x2, op0=op0, op1=op1), [in0, s1, s2], [out])

    def stt(self, eng, out, in0, scalar, in1, op0, op1):
        o, a, s, b = out.ap, in0.ap, self._a(scalar), in1.ap
        return self.op(eng, lambda e: e.scalar_tensor_tensor(out=o, in0=a, scalar=s, in1=b, op0=op0, op1=op1), [in0, scalar, in1], [out])

    def copy(self, eng, out, in_):
        o, i = out.ap, in_.ap
        if eng == "act":
            return self.op("act", lambda e: e.copy(out=o, in_=i), [in_], [out])
        return self.op(eng, lambda e: e.tensor_copy(out=o, in_=i), [in_], [out])

    def memset(self, eng, out, val):
        o = out.ap
        return self.op(eng, lambda e: e.memset(o, val), [], [out])

    def scan(self, out, d0, d1, init, op0, op1):
        o, a, b, i = out.ap, d0.ap, d1.ap, self._a(init)
        return self.op("dve", lambda e: e.tensor_tensor_scan(out=o, data0=a, data1=b, initial=i, op0=op0, op1=op1), [d0, d1, init], [out])

    def recip(self, out, in_):
        o, i = out.ap, in_.ap
        return self.op("dve", lambda e: e.reciprocal(out=o, in_=i), [in_], [out])

    def max8(self, out, in_):
        o, i = out.ap, in_.ap
        return self.op("dve", lambda e: e.max(out=o, in_=i), [in_], [out])

    def bn_stats(self, out, in_):
        o, i = out.ap, in_.ap
        return self.op("dve", lambda e: e.bn_stats(out=o, in_=i), [in_], [out])

    def bn_aggr(self, out, in_):
        o, i = out.ap, in_.ap
        return self.op("dve", lambda e: e.bn_aggr(out=o, in_=i), [in_], [out])


def DV(ap, name, region=0):
    return View(ap, [("d", name, region)])


class MK:
    def __init__(self, NSEQ=2, S=2048, layers=None, dbg=None):
        self.NSEQ, self.S = NSEQ, S
        self.NTOK = NSEQ * S
        self.NCT = self.NTOK // CT
        self.CPS = S // CT
        self.layers = layers if layers is not None else [(0, 0), (1, 0), (2, 0), (0, 1)]
        self.L = len(self.layers)
        self.nA = sum(1 for k, _ in self.layers if k == 0)
        self.nB = sum(1 for k, _ in self.layers if k == 1)
        self.nC = sum(1 for k, _ in self.layers if k == 2)
        self.dbg = dbg
        nc = self.nc = bass.Bass("TRN2", target_bir_lowering=False)
        self.P = Prog(nc, arena_kib=176)
        self.inputs = {}
        self.build()

    def inp(self, name, shape, dt=F32):
        ap = self.nc.dram_tensor(name, list(shape), dt, kind="ExternalInput").ap()
        self.inputs[name] = (tuple(shape), ap)
        return ap

    def build(self):
        P, L, NT = self.P, self.L, self.NTOK
        nA, nB, nC = max(self.nA, 1), max(self.nB, 1), max(self.nC, 1)
        self.xT = self.inp("xT", [D, NT])
        self.cT = self.inp("cT", [128, KT, self.NSEQ])
        self.ada_w = self.inp("ada_w", [L, D, 6 * D])
        self.ada_bT = self.inp("ada_bT", [128, L, 96])
        self.lnp = self.inp("lnp", [128, L, 4, KT])
        self.ident_d = self.inp("ident", [128, 128])
        self.maskneg_d = self.inp("maskneg", [128, 4, 512])
        self.rg_w_in = self.inp("rg_w_in", [nA, D, 2 * RGW])
        self.rg_vec = self.inp("rg_vec", [128, nA, 9, RGT])
        self.rg_slabs = self.inp("rg_slabs", [nA, RGT, 128, 2, 4, 128])
        self.rg_w_out = self.inp("rg_w_out", [nA, RGW, D])
        self.sg_w_in = self.inp("sg_w_in", [nB, D, 2 * D])
        self.sg_vecp = self.inp("sg_vecp", [128, nB, 2, KT])
        self.sg_rows = self.inp("sg_rows", [nB, 4, D])
        self.sg_w_spT = self.inp("sg_w_spT", [nB, 128, 8, 128])
        self.sg_w_out = self.inp("sg_w_out", [nB, D, D])
        self.ml_w_in = self.inp("ml_w_in", [nC, D, 2 * D])
        self.ml_vecp = self.inp("ml_vecp", [128, nC, 7, KT])
        self.ml_bd = self.inp("ml_bd", [nC, 3, 128, KT, 128])
        self.ml_w_gates = self.inp("ml_w_gates", [nC, 128, 48, 16])
        self.ml_b_gates = self.inp("ml_b_gates", [8, nC, 2])
        self.ml_w_out = self.inp("ml_w_out", [nC, D, D])
        self.moe_w_router = self.inp("moe_w_router", [L, 128, KT, NE])
        self.moe_b_router = self.inp("moe_b_router", [L, NE, 1])
        self.moe_w_gate_up = self.inp("moe_w_gate_up", [L, NE, D, 2 * DE])
        self.moe_b_gu = self.inp("moe_b_gu", [128, L, NE, 8])
        self.moe_w_down = self.inp("moe_w_down", [L, NE, DE, D])
        self.moe_b_down = self.inp("moe_b_down", [L, NE, D])
        self.outT = self.nc.dram_tensor("outT", [D, NT], F32, kind="ExternalOutput").ap()
        self.X1 = P.dram("X1", [D, NT], F32)
        self.X2 = P.dram("X2", [D, NT], F32)
        self.YT = P.dram("YT", [D, NT], F32)

        self.setup_consts()
        self.compute_mods()
        xin = self.xT
        xin_name = "xT"
        for li, (kind, j) in enumerate(self.layers):
            last = li == self.L - 1
            if kind == 0:
                self.rg_layer(li, j, xin, xin_name)
            elif kind == 1:
                self.sgu_layer(li, j, xin, xin_name)
            else:
                self.ml_layer(li, j, xin, xin_name)
            dst, dname = (self.outT, "outT") if last else (self.X2, "X2")
            self.moe_layer(li, dst, dname)
            xin, xin_name = self.X2, "X2"
        evs = []
        for k, e in P.lw.items():
            if k[0] == "d" and k[1] == "outT":
                evs.append(e)
        P.wait_all("sp", evs)
        P.emit()

    def dv(self, ap, name, ct):
        return DV(ap, name, ct)

    def setup_consts(self):
        P = self.P
        self.ident = P.sb("identc", [128, 128], F32)
        P.dma("sp", self.ident, DV(self.ident_d, "ident"))
        self.onesD = P.sb("onesD", [128, 128], F32)
        P.memset("pool", self.onesD, 1.0 / D)
        self.onec = P.sb("onec", [128, 1], F32)
        P.memset("pool", self.onec, 1.0)
        self.epsc = P.sb("epsc", [128, 2], F32)
        P.memset("pool", self.epsc[:, 0:1], LN_EPS / (ALPHA * ALPHA))
        P.memset("pool", self.epsc[:, 1:2], LN_EPS)
        self.MOD = P.sb("MOD", [128, self.L, 6, KT, self.NSEQ], F32)
        self.LNP = P.sb("LNP", [128, self.L, 4, KT], F32)
        P.dma("sp", self.LNP, DV(self.lnp, "lnp"))
        self.SEL = P.sb("SEL", [32, 2, 128], F32)
        self.lnmean = P.sb("lnmean", [128, 512], F32)
        self.lnrstd = P.sb("lnrstd", [128, 512], F32)
        self.lntmp = P.sb("lntmp", [128, 2, 512], F32)

    def compute_mods(self):
        P, L, NS = self.P, self.L, self.NSEQ
        m = P.mark()
        cT = P.alloc([KT, NS], F32)
        P.dma("sp", cT, DV(self.cT, "cT"))
        cond = P.alloc([KT, NS], F32)
        P.act(cond, cT, AF.Silu)
        abT = P.alloc([L, 96], F32)
        P.dma("sp", abT, DV(self.ada_bT, "ada_bT"))
        wb = [P.alloc([D], F32) for _ in range(3)]
        acc = P.alloc([KT, NS], F32)
        n = 0
        for l in range(L):
            for c6 in range(6):
                for k in range(KT):
                    w = wb[n % 3]
                    n += 1
                    P.dma("sp", w, DV(self.ada_w[l, k * 128:(k + 1) * 128, c6 * D:(c6 + 1) * D], "ada_w"))
                    ps = P.ps(k % 2)
                    for f in range(KT):
                        P.mm(ps[:, f * NS:(f + 1) * NS], w[:, f * 128:(f + 1) * 128], cond[:, k, :])
                    pv = ps[:, 0:KT * NS].re("p (f s) -> p f s", s=NS)
                    if k == 0:
                        P.copy("dve", acc, pv)
                    else:
                        P.tt("dve", acc, acc, pv, ALU.add)
                bias = abT[:, l, c6 * KT:(c6 + 1) * KT]
                bb = View(bias.ap.unsqueeze(2).broadcast_to([128, KT, NS]), bias.keys)
                dst = self.MOD[:, l, c6]
                if c6 in (1, 4):
                    P.stt("dve", dst, acc, 1.0, bb, ALU.add, ALU.add)
                elif c6 in (2, 5):
                    P.tt("dve", dst, acc, bb, ALU.add)
                    P.ts("dve", dst, dst, 1.0, ALU.add, 1.0 / ALPHA, ALU.mult)
                else:
                    P.tt("dve", dst, acc, bb, ALU.add)
        P.release(m)

    def mod(self, l, c6, k, s):
        return self.MOD[:, l, c6, k, s:s + 1]

    def resid_ln(self, li, which, ct, V, dst, dname):
        P = self.P
        psm, pse = P.ps(6), P.ps(7)
        for k in range(KT):
            P.mm(psm, self.onesD, V[k], start=(k == 0), stop=(k == KT - 1))
        for k in range(KT):
            sq = self.lntmp[:, k % 2]
            P.act(sq, V[k], AF.Square)
            P.mm(pse, self.onesD, sq, start=(k == 0), stop=(k == KT - 1))
        mean, rstd = self.lnmean, self.lnrstd
        P.copy("act", mean, psm)
        m2 = self.lntmp[:, 0]
        P.tt("pool", m2, mean, mean, ALU.mult)
        P.tt("dve", rstd, pse, m2, ALU.subtract)
        P.act(rstd, rstd, AF.Sqrt, bias=self.epsc[:, 0:1])
        P.recip(rstd, rstd)
        gi = 0 if which == 1 else 2
        for k in range(KT):
            P.tt("dve", V[k], V[k], mean, ALU.subtract)
            P.tt("pool", V[k], V[k], rstd, ALU.mult)
            P.act(V[k], V[k], AF.Identity, bias=self.LNP[:, li, gi + 1, k:k + 1], scale=self.LNP[:, li, gi, k:k + 1])
            P.dma("sp", self.dv(dst[k * 128:(k + 1) * 128, ct * CT:(ct + 1) * CT], dname, ct), V[k])

    def load_x_tiles(self, src, sname, ct, X):
        for k in range(KT):
            self.P.dma("sp", X[k], self.dv(src[k * 128:(k + 1) * 128, ct * CT:(ct + 1) * CT], sname, ct))

    def modulate(self, li, which, ct, X, H):
        s = ct // self.CPS
        c0 = 0 if which == 1 else 3
        for k in range(KT):
            self.P.act(H[k], X[k], AF.Identity, bias=self.mod(li, c0, k, s), scale=self.mod(li, c0 + 1, k, s))

    def stream(self, WB, items, body):
        def ld(i):
            src, kt, name = items[i]
            self.wload(WB[i % 2][:, 0:kt], src, kt, name)
        ld(0)
        for i in range(len(items)):
            if i + 1 < len(items):
                ld(i + 1)
            body(i, WB[i % 2])

    def wload(self, buf, src_ap, kt, name):
        step = 8
        for k0 in range(0, kt, step):
            k1 = min(kt, k0 + step)
            self.P.dma("pool", buf[:, k0:k1], DV(src_ap[k0 * 128:k1 * 128, :].rearrange("(k p) n -> p k n", p=128), name))

    def moe_layer(self, li, dst, dname):
        P = self.P
        TG = 2 * CT
        for g in range(self.NTOK // TG):
            m0 = P.mark()
            cts = (2 * g, 2 * g + 1)
            H2 = [P.alloc([TG], BF16) for _ in range(KT)]
            ACC = [[P.alloc([CT], F32) for _ in range(2)] for _ in range(KT)]
            CBT = P.alloc([TG], F32)
            BGU = P.alloc([NE, 8], F32)
            P.dma("sp", BGU, DV(self.moe_b_gu[:, li], "moe_b_gu"))
            P.ts("dve", BGU[:, :, 4:8], BGU[:, :, 4:8], 1.0, ALU.add)
            m1 = P.mark()
            WR = P.alloc([KT, NE], F32)
            P.dma("sp", WR, DV(self.moe_w_router[li], "moe_w_router"))
            BR = P.alloc([1], F32)
            P.dma("sp", BR[0:32], DV(self.moe_b_router[li], "moe_b_router"))
            BD = P.alloc([D], F32)
            P.dma("sp", BD[0:32], DV(self.moe_b_down[li], "moe_b_down"))
            XS = [P.alloc([TG], F32) for _ in range(2)]
            HF = [P.alloc([TG], F32) for _ in range(2)]
            for k in range(KT):
                xs, hf = XS[k % 2], HF[k % 2]
                for c in range(2):
                    ct = cts[c]
                    s = ct // self.CPS
                    P.dma("sp", xs[:, c * CT:(c + 1) * CT], self.dv(self.X1[k * 128:(k + 1) * 128, ct * CT:(ct + 1) * CT], "X1", ct))
                    P.act(hf[:, c * CT:(c + 1) * CT], xs[:, c * CT:(c + 1) * CT], AF.Identity,
                          bias=self.mod(li, 3, k, s), scale=self.mod(li, 4, k, s))
                P.copy("dve", H2[k], hf)
                for c in range(2):
                    P.mm(P.ps(c)[0:32], WR[:, k, :], hf[:, c * CT:(c + 1) * CT], start=(k == 0), stop=(k == KT - 1))
            LT = P.alloc([TG], F32)
            for c in range(2):
                P.act(LT[0:32, c * CT:(c + 1) * CT], P.ps(c)[0:32], AF.Identity, bias=BR[0:32, 0:1])
            NTT = TG // 128
            LG = P.alloc([NTT, NE], F32)
            pt = P.ps(2)
            for tt in range(NTT):
                P.tr(pt[:, tt * NE:(tt + 1) * NE], LT[0:32, tt * 128:(tt + 1) * 128], self.ident[0:32, 0:32])
            P.copy("dve", LG, pt[:, 0:NTT * NE].re("p (t e) -> p t e", e=NE))
            M8 = P.alloc([NTT, 8], F32)
            for tt in range(NTT):
                P.max8(M8[:, tt, :], LG[:, tt, :])
            MSK = P.alloc([NTT, NE], F32)
            EX = P.alloc([NTT, NE], F32)
            NM = P.alloc([NTT], F32)
            P.ts("dve", NM, M8[:, :, 0], -1.0, ALU.mult)
            for tt in range(NTT):
                P.ts("dve", MSK[:, tt, :], LG[:, tt, :], M8[:, tt, 3:4], ALU.is_ge)
                P.act(EX[:, tt, :], LG[:, tt, :], AF.Exp, bias=NM[:, tt:tt + 1])
            P.tt("dve", EX, EX, MSK, ALU.mult)
            SM = P.alloc([NTT], F32)
            sm_o, ex_i = SM.ap, EX.ap
            P.op("dve", lambda e: e.tensor_reduce(out=sm_o, in_=ex_i, axis=mybir.AxisListType.X, op=ALU.add), [EX], [SM])
            P.recip(SM, SM)
            P.tt("dve", EX, EX, View(SM.ap.unsqueeze(2).broadcast_to([128, NTT, NE]), SM.keys), ALU.mult)
            for tt in range(NTT):
                c = tt // 4
                P.tr(P.ps(c)[0:32, (tt % 4) * 128:(tt % 4 + 1) * 128], EX[:, tt, :], self.ident)
            for c in range(2):
                P.copy("act", CBT[0:32, c * CT:(c + 1) * CT], P.ps(c)[0:32])
            n = 0
            for f in range(KT):
                for c in range(2):
                    ps = P.ps(4 + n % 2)
                    n += 1
                    P.mm(ps, BD[0:32, f * 128:(f + 1) * 128], CBT[0:32, c * CT:(c + 1) * CT])
                    P.copy("act", ACC[f][c], ps)
            P.release(m1)
            WGU = [P.alloc([KT, 2, 256], BF16) for _ in range(2)]
            WDN = P.alloc([4, D], BF16)
            A = [[P.alloc([CT], BF16) for _ in range(4)] for _ in range(2)]
            CBE = [P.alloc([CT], F32) for _ in range(2)]
            Gb = [P.alloc([CT], F32) for _ in range(2)]
            Ub = [P.alloc([CT], F32) for _ in range(2)]
            Sb = [P.alloc([CT], F32) for _ in range(2)]
            nw = 0
            nm = 0
            nd = 0
            def load_chunk(n):
                e_, mp_ = divmod(n, 2)
                w_ = WGU[n % 2]
                wsrc_ = self.moe_w_gate_up[li, e_]
                for gu in range(2):
                    col0 = gu * DE + mp_ * 256
                    for k0 in (0, 8):
                        P.dma("pool", w_[:, k0:k0 + 8, gu, :],
                              DV(wsrc_[k0 * 128:(k0 + 8) * 128, col0:col0 + 256].rearrange("(k p) n -> p k n", p=128), "moe_w_gate_up"))
            load_chunk(0)
            self.wload(WDN, self.moe_w_down[li, 0], 4, "moe_w_down")
            for e in range(NE):
                wsrc = self.moe_w_gate_up[li, e]
                sel = self.SEL[:, e % 2, :]
                P.copy("pool", sel, View(self.ident.ap[0:32, e:e + 1].broadcast_to([32, 128]), self.ident.keys))
                for c in range(2):
                    ps = P.ps(6)
                    P.mm(ps, sel, CBT[0:32, c * CT:(c + 1) * CT])
                    P.copy("act", CBE[c], ps)
                for mp in range(2):
                    w = WGU[nw % 2]
                    if mp == 1 and e > 0:
                        self.wload(WDN, self.moe_w_down[li, e], 4, "moe_w_down")
                    if nw + 1 < 2 * NE:
                        load_chunk(nw + 1)
                    nw += 1
                    for c in range(2):
                        for mi in range(2):
                            mt = mp * 2 + mi
                            psG, psU = P.ps(nm % 2), P.ps(2 + nm % 2)
                            G, U, S_ = Gb[nm % 2], Ub[nm % 2], Sb[nm % 2]
                            nm += 1
                            for k in range(KT):
                                P.mm(psG, w[:, k, 0, mi * 128:(mi + 1) * 128], H2[k][:, c * CT:(c + 1) * CT], start=(k == 0), stop=(k == KT - 1))
                            for k in range(KT):
                                P.mm(psU, w[:, k, 1, mi * 128:(mi + 1) * 128], H2[k][:, c * CT:(c + 1) * CT], start=(k == 0), stop=(k == KT - 1))
                            P.ts("dve", G, psG, BGU[:, e, mt:mt + 1], ALU.add, 7.0, ALU.min)
                            P.act(S_, G, AF.Sigmoid, scale=1.702)
                            P.ts("dve", U, psU, BGU[:, e, 4 + mt:5 + mt], ALU.add, 8.0, ALU.min)
                            P.stt("dve", U, U, -6.0, CBE[c], ALU.max, ALU.mult)
                            P.tt("pool", G, G, S_, ALU.mult)
                            P.tt("pool", A[c][mt], G, U, ALU.mult)
                for c in range(2):
                    for f in range(KT):
                        ps = P.ps(4 + nd % 2)
                        nd += 1
                        for k in range(4):
                            P.mm(ps, WDN[:, k, f * 128:(f + 1) * 128], A[c][k], start=(k == 0), stop=(k == 3))
                        P.tt("dve", ACC[f][c], ACC[f][c], ps, ALU.add)
            for c in range(2):
                ct = cts[c]
                s = ct // self.CPS
                V = []
                for f in range(KT):
                    xs = XS[f % 2][:, c * CT:(c + 1) * CT] if False else None
                    xt = CBE[0] if False else None
                    V.append(ACC[f][c])
                xbuf = [Gb[0], Gb[1], Ub[0], Ub[1]]
                for f in range(KT):
                    xb = xbuf[f % 4]
                    P.dma("sp", xb, self.dv(self.X1[f * 128:(f + 1) * 128, ct * CT:(ct + 1) * CT], "X1", ct))
                    P.stt("dve", V[f], V[f], self.mod(li, 5, f, s), xb, ALU.mult, ALU.add)
                self.resid_ln(li, 2, ct, V, dst, dname)
            P.release(m0)

    def rg_slab_tiles(self, j):
        b0, b1 = (128 * j) // 160, (128 * j + 127) // 160
        i0, i1 = (160 * b0) // 128, (160 * b1 + 159) // 128
        return list(range(i0, i1 + 1))

    def rg_layer(self, li, j, xsrc, xname):
        P = self.P
        m0 = P.mark()
        VEC = P.sb(f"rgvec{li}", [128, 9, RGT], F32)
        P.dma("sp", VEC, DV(self.rg_vec[:, j], "rg_vec"))
        cfe = VEC[:, 8, :]
        P.act(cfe, VEC[:, 7, :], AF.Exp, scale=-1.0)
        P.act(cfe, cfe, AF.Ln, bias=1.0)
        P.ts("dve", cfe, cfe, -8.0, ALU.mult)
        HST = P.sb(f"rghst{li}", [128, RGT], F32)
        HALO = P.sb(f"rghalo{li}", [128, RGT, 3], F32)
        X = [P.alloc([CT], F32) for _ in range(KT)]
        H = [P.alloc([CT], BF16) for _ in range(KT)]
        WB = [P.alloc([RGT, CT], BF16) for _ in range(2)]
        GATE = [P.alloc([CT], BF16) for _ in range(RGT)]
        XRB = [P.alloc([CT], BF16) for _ in range(RGT)]
        XRF = [P.alloc([CT], F32) for _ in range(2)]
        RECB = [P.alloc([CT + 4], F32) for _ in range(2)]
        SL = [P.alloc([2, 4, 128], BF16) for _ in range(2)]
        Rb = [P.alloc([CT], F32) for _ in range(2)]
        Ib = [P.alloc([CT], F32) for _ in range(2)]
        Ab = [P.alloc([CT], F32) for _ in range(2)]
        Tb = [P.alloc([CT], F32) for _ in range(2)]
        HS = [P.alloc([CT], F32) for _ in range(2)]
        nw = 0
        npz = 0
        for ct in range(self.NCT):
            s = ct // self.CPS
            if ct % self.CPS == 0:
                P.memset("pool", HST, 0.0)
                P.memset("pool", HALO, 0.0)
            self.load_x_tiles(xsrc, xname, ct, X)
            self.modulate(li, 1, ct, X, H)
            def body_in(nb, w):
                nonlocal npz
                for mi in range(4):
                    ft = nb * 4 + mi
                    ps = P.ps(npz % 4)
                    npz += 1
                    for k in range(KT):
                        P.mm(ps, w[:, k, mi * 128:(mi + 1) * 128], H[k], start=(k == 0), stop=(k == KT - 1))
                    if ft < RGT:
                        P.act(GATE[ft], ps, AF.Gelu_apprx_tanh)
                    else:
                        jj = ft - RGT
                        rb = RECB[jj % 2]
                        xr = XRF[jj % 2]
                        P.copy("act", rb[:, 3:3 + CT], ps)
                        P.copy("pool", rb[:, 0:3], HALO[:, jj, :])
                        P.copy("pool", HALO[:, jj, :], rb[:, CT:CT + 3])
                        P.act(xr, rb[:, 3:3 + CT], AF.Identity, bias=VEC[:, 4, jj:jj + 1], scale=VEC[:, 3, jj:jj + 1])
                        for d in (1, 2, 3):
                            P.stt("dve", xr, rb[:, 3 - d:3 - d + CT], VEC[:, 3 - d, jj:jj + 1], xr, ALU.mult, ALU.add)
                        P.copy("act", XRB[jj], xr)
            self.stream(WB, [(self.rg_w_in[j][:, nb * CT:(nb + 1) * CT], KT, "rg_w_in") for nb in range(10)], body_in)
            P.dma("pool", SL[0], DV(self.rg_slabs[j, 0], "rg_slabs"))
            for jt in range(RGT):
                tiles = self.rg_slab_tiles(jt)
                sl = SL[jt % 2]
                if jt + 1 < RGT:
                    P.dma("pool", SL[(jt + 1) % 2], DV(self.rg_slabs[j, jt + 1], "rg_slabs"))
                psR, psI = P.ps(4 + 2 * (jt % 2)), P.ps(5 + 2 * (jt % 2))
                for gi, ps in ((0, psR), (1, psI)):
                    for si, it in enumerate(tiles):
                        P.mm(ps, sl[:, gi, si, :], XRB[it], start=(si == 0), stop=(si == len(tiles) - 1))
                R, I, A_, T, hs = Rb[jt % 2], Ib[jt % 2], Ab[jt % 2], Tb[jt % 2], HS[jt % 2]
                P.act(R, psR, AF.Sigmoid, bias=VEC[:, 5, jt:jt + 1])
                P.act(I, psI, AF.Sigmoid, bias=VEC[:, 6, jt:jt + 1])
                P.act(A_, R, AF.Exp, scale=VEC[:, 8, jt:jt + 1])
                P.tt("pool", T, A_, A_, ALU.mult)
                P.ts("dve", T, T, -1.0, ALU.mult, 1.0, ALU.add)
                P.act(T, T, AF.Sqrt)
                P.tt("pool", I, I, XRB[jt], ALU.mult)
                P.tt("pool", T, T, I, ALU.mult)
                P.scan(hs, A_, T, HST[:, jt:jt + 1], ALU.mult, ALU.add)
                P.copy("act", HST[:, jt:jt + 1], hs[:, CT - 1:CT])
                P.tt("dve", GATE[jt], GATE[jt], hs, ALU.mult)
            def body_out(nb, w):
                nonlocal npz
                for mi in range(4):
                    ft = nb * 4 + mi
                    ps = P.ps(npz % 4)
                    npz += 1
                    for k in range(RGT):
                        P.mm(ps, w[:, k, mi * 128:(mi + 1) * 128], GATE[k], start=(k == 0), stop=(k == RGT - 1))
                    P.stt("dve", X[ft], ps, self.mod(li, 2, ft, s), X[ft], ALU.mult, ALU.add)
            self.stream(WB, [(self.rg_w_out[j][:, nb * CT:(nb + 1) * CT], RGT, "rg_w_out") for nb in range(4)], body_out)
            self.resid_ln(li, 1, ct, X, self.X1, "X1")
        P.release(m0)

    def bcast_rows(self, dst, src_ap, name):
        n = src_ap.shape[-1]
        self.P.dma("sp", dst, DV(src_ap.broadcast_to([128, n]), name))

    def sgu_layer(self, li, j, xsrc, xname):
        P = self.P
        m0 = P.mark()
        VP = P.sb(f"sgvp{li}", [128, 2, KT], F32)
        P.dma("sp", VP, DV(self.sg_vecp[:, j], "sg_vecp"))
        BIASV = P.alloc([D], F32)
        LNG = P.alloc([D], F32)
        LNB = P.alloc([D], F32)
        BSP = P.alloc([8, 128], F32)
        self.bcast_rows(BIASV, self.sg_rows[j, 0:1, :], "sg_rows")
        self.bcast_rows(LNG, self.sg_rows[j, 1:2, :], "sg_rows")
        self.bcast_rows(LNB, self.sg_rows[j, 2:3, :], "sg_rows")
        self.bcast_rows(BSP.re("p g i -> p (g i)"), self.sg_rows[j, 3:4, 0:1024], "sg_rows")
        WSTf = P.alloc([8, 128], F32)
        WSTb = P.alloc([8, 128], BF16)
        P.dma("sp", WSTf, DV(self.sg_w_spT[j], "sg_w_spT"))
        P.memset("pool", WSTf[64:128, :, 0:64], 0.0)
        P.copy("pool", WSTb, WSTf)
        H = [P.alloc([CT], BF16) for _ in range(KT)]
        WB = [P.alloc([KT, CT], BF16) for _ in range(2)]
        U = [P.alloc([CT], BF16) for _ in range(KT)]
        VT = [P.alloc([D], F32) for _ in range(4)]
        X = [P.alloc([CT], F32, at=VT[0].off + f * 2048) for f in range(KT)]
        VN = [P.alloc([D], BF16) for _ in range(4)]
        XS = [P.alloc([CT], F32) for _ in range(2)]
        T = [P.alloc([CT], F32) for _ in range(2)]
        ST = P.alloc([4, 6], F32)
        MV = P.alloc([4], F32)
        nw = 0
        npz = 0
        for ct in range(self.NCT):
            s = ct // self.CPS
            for k in range(KT):
                xs = XS[k % 2]
                P.dma("sp", xs, self.dv(xsrc[k * 128:(k + 1) * 128, ct * CT:(ct + 1) * CT], xname, ct))
                P.act(H[k], xs, AF.Identity, bias=self.mod(li, 0, k, s), scale=self.mod(li, 1, k, s))
            def body_uv(nb, w):
                nonlocal npz
                if nb < 4:
                    for mi in range(4):
                        ft = nb * 4 + mi
                        ps = P.ps(npz % 4)
                        npz += 1
                        for k in range(KT):
                            P.mm(ps, w[:, k, mi * 128:(mi + 1) * 128], H[k], start=(k == 0), stop=(k == KT - 1))
                        P.act(U[ft], ps, AF.Gelu_apprx_tanh, bias=VP[:, 0, ft:ft + 1])
                    return
                nb -= 4
                for tt in range(4):
                    ps = P.ps(npz % 4)
                    npz += 1
                    for k in range(KT):
                        P.mm(ps, H[k][:, tt * 128:(tt + 1) * 128], w[:, k, :], start=(k == 0), stop=(k == KT - 1))
                    vt = VT[tt][:, nb * CT:(nb + 1) * CT]
                    P.tt("dve", vt, ps, BIASV[:, nb * CT:(nb + 1) * CT], ALU.add)
                    P.act(vt, vt, AF.Gelu_apprx_tanh)
            self.stream(WB, [(self.sg_w_in[j][:, nb * CT:(nb + 1) * CT], KT, "sg_w_in") for nb in range(8)], body_uv)
            for tt in range(4):
                P.reduce_add(MV[:, 0:1], VT[tt])
                P.act(VN[tt], VT[tt], AF.Square, accum=MV[:, 1:2])
                P.ts("dve", MV[:, 0:1], MV[:, 0:1], 1.0 / D, ALU.mult)
                P.tt("dve", MV[:, 3:4], MV[:, 0:1], MV[:, 0:1], ALU.mult)
                P.stt("dve", MV[:, 1:2], MV[:, 1:2], 1.0 / D, MV[:, 3:4], ALU.mult, ALU.subtract)
                P.act(MV[:, 2:3], MV[:, 1:2], AF.Sqrt, bias=self.epsc[:, 1:2])
                P.recip(MV[:, 2:3], MV[:, 2:3])
                P.ts("dve", VT[tt], VT[tt], MV[:, 0:1], ALU.subtract, MV[:, 2:3], ALU.mult)
                P.tt("pool", VT[tt], VT[tt], LNG, ALU.mult)
                P.tt("pool", VN[tt], VT[tt], LNB, ALU.add)
            for c in range(KT):
                g = c // 2
                ps = P.ps(4 + c % 2)
                for tt in range(4):
                    P.mm(ps[:, tt * 128:(tt + 1) * 128], VN[tt][:, c * 128:(c + 1) * 128], WSTb[:, g, :])
                t = T[c % 2]
                bsp = BSP[:, g, :]
                P.tt("dve", t.re("p (a i) -> p a i", i=128), ps.re("p (a i) -> p a i", i=128),
                     View(bsp.ap.unsqueeze(1).broadcast_to([128, 4, 128]), bsp.keys), ALU.add)
                P.tt("pool", U[c], U[c], t, ALU.mult)
            for f in range(KT):
                P.dma("sp", X[f], self.dv(xsrc[f * 128:(f + 1) * 128, ct * CT:(ct + 1) * CT], xname, ct))
            def body_o(nb, w):
                nonlocal npz
                for mi in range(4):
                    ft = nb * 4 + mi
                    ps = P.ps(npz % 4)
                    npz += 1
                    for k in range(KT):
                        P.mm(ps, w[:, k, mi * 128:(mi + 1) * 128], U[k], start=(k == 0), stop=(k == KT - 1))
                    t = T[ft % 2]
                    P.act(t, ps, AF.Identity, bias=VP[:, 1, ft:ft + 1])
                    P.stt("dve", X[ft], t, self.mod(li, 2, ft, s), X[ft], ALU.mult, ALU.add)
            self.stream(WB, [(self.sg_w_out[j][:, nb * CT:(nb + 1) * CT], KT, "sg_w_out") for nb in range(4)], body_o)
            self.resid_ln(li, 1, ct, X, self.X1, "X1")
        P.release(m0)

    def ml_layer(self, li, j, xsrc, xname):
        P = self.P
        S, NT, NS = self.S, self.CPS, self.NSEQ
        NTT = S // 128
        if not hasattr(self, "ml_scr"):
            self.ml_scr = dict(
                QT=P.dram("mlQT", [D, self.NTOK], BF16), KTs=P.dram("mlKT", [D, self.NTOK], BF16),
                VTK=P.dram("mlVTK", [self.NTOK, D], BF16), XC=P.dram("mlXC", [D, self.NTOK], F32),
                SZ=P.dram("mlSZ", [D, self.NTOK], F32))
            self.ONES256 = P.sb("ones256", [128, 128], F32)
            P.memset("pool", self.ONES256, 1.0 / 256)
            self.ONESB = P.sb("onesb", [128, 128], BF16)
            P.memset("pool", self.ONESB, 1.0)
            self.SELH = P.sb("selh", [8, 8, 128], F32)
            P.copy("pool", self.SELH, View(self.ident.ap[0:8, 0:8].unsqueeze(2).broadcast_to([8, 8, 128]), self.ident.keys))
        scr = self.ml_scr
        m0 = P.mark()
        VP = P.sb(f"mlvp{li}", [128, 7, KT], F32)
        P.dma("sp", VP, DV(self.ml_vecp[:, j], "ml_vecp"))
        BG = P.sb(f"mlbg{li}", [8, 3], F32)
        P.dma("sp", BG[:, 0:2], DV(self.ml_b_gates[:, j], "ml_b_gates"))
        P.ts("dve", BG[:, 2:3], BG[:, 1:2], -1.0, ALU.mult)
        HALO = P.sb(f"mlhalo{li}", [128, KT, 3], F32)
        BD = [P.alloc([KT, 128], BF16) for _ in range(3)]
        for qi in range(3):
            self.P.dma("pool", BD[qi], DV(self.ml_bd[j, qi], "ml_bd"))
        WG = P.alloc([48, 16], BF16)
        P.dma("pool", WG, DV(self.ml_w_gates[j], "ml_w_gates"))
        MASK = P.alloc([4, 512], F32)
        P.dma("sp", MASK, DV(self.maskneg_d, "maskneg"))
        GI = P.alloc([S], F32)
        LF = P.alloc([S], F32)
        BT = P.alloc([S], F32)
        CTK = P.alloc([NTT, 8], F32)
        m1 = P.mark()
        for s in range(NS):
            P.release(m1)
            H = [P.alloc([CT], BF16) for _ in range(KT)]
            WB = [P.alloc([KT, CT], BF16) for _ in range(2)]
            XCB = [P.alloc([CT], BF16) for _ in range(KT)]
            XMB = [P.alloc([CT], BF16) for _ in range(KT)]
            XS = [P.alloc([CT], F32) for _ in range(2)]
            RECB = [P.alloc([CT + 4], F32) for _ in range(2)]
            XCF = [P.alloc([CT], F32) for _ in range(2)]
            QB = [P.alloc([CT], BF16) for _ in range(3)]
            VST = [P.alloc([4, 128], BF16) for _ in range(2)]
            TG_ = [P.alloc([CT], F32) for _ in range(2)]
            nw = 0
            npz = 0
            P.memset("pool", HALO, 0.0)
            for c in range(NT):
                ct = s * NT + c
                for k in range(KT):
                    xs = XS[k % 2]
                    P.dma("sp", xs, self.dv(xsrc[k * 128:(k + 1) * 128, ct * CT:(ct + 1) * CT], xname, ct))
                    P.act(H[k], xs, AF.Identity, bias=self.mod(li, 0, k, s), scale=self.mod(li, 1, k, s))
                def body_in(nb, w):
                    nonlocal npz
                    for mi in range(4):
                        ft = nb * 4 + mi
                        ps = P.ps(npz % 4)
                        npz += 1
                        for k in range(KT):
                            P.mm(ps, w[:, k, mi * 128:(mi + 1) * 128], H[k], start=(k == 0), stop=(k == KT - 1))
                        if ft < KT:
                            rb, xc = RECB[ft % 2], XCF[ft % 2]
                            P.copy("act", rb[:, 3:3 + CT], ps)
                            P.copy("pool", rb[:, 0:3], HALO[:, ft, :])
                            P.copy("pool", HALO[:, ft, :], rb[:, CT:CT + 3])
                            P.copy("pool", XMB[ft], rb[:, 3:3 + CT])
                            P.act(xc, rb[:, 3:3 + CT], AF.Identity, bias=VP[:, 4, ft:ft + 1], scale=VP[:, 3, ft:ft + 1])
                            for d in (1, 2, 3):
                                P.stt("dve", xc, rb[:, 3 - d:3 - d + CT], VP[:, 3 - d, ft:ft + 1], xc, ALU.mult, ALU.add)
                            P.act(xc, xc, AF.Silu)
                            P.copy("pool", XCB[ft], xc)
                            P.dma("sp", self.dv(scr["XC"][ft * 128:(ft + 1) * 128, ct * CT:(ct + 1) * CT], "mlXC", ct), xc)
                        else:
                            f2 = ft - KT
                            t = TG_[f2 % 2]
                            P.act(t, ps, AF.Sigmoid)
                            P.dma("sp", self.dv(scr["SZ"][f2 * 128:(f2 + 1) * 128, ct * CT:(ct + 1) * CT], "mlSZ", ct), t)
                self.stream(WB, [(self.ml_w_in[j][:, nb * CT:(nb + 1) * CT], KT, "ml_w_in") for nb in range(8)], body_in)
                psGI, psGF = P.ps(6), P.ps(7)
                ng = 0
                for ft in range(KT):
                    for qi, src, dname in ((0, XCB, "QT"), (1, XCB, "KTs"), (2, XMB, None)):
                        ps = P.ps(npz % 4)
                        npz += 1
                        P.mm(ps, BD[qi][:, ft, :], src[ft])
                        qb = QB[qi]
                        P.copy("act" if qi != 1 else "dve", qb, ps)
                        if dname is not None:
                            P.dma("sp", self.dv(scr[dname][ft * 128:(ft + 1) * 128, ct * CT:(ct + 1) * CT], "ml" + dname, ct), qb)
                        P.mm(psGI[0:8], WG[:, qi * KT + ft, 0:8], qb, start=(ng == 0), stop=(ng == 47))
                        P.mm(psGF[0:8], WG[:, qi * KT + ft, 8:16], qb, start=(ng == 0), stop=(ng == 47))
                        ng += 1
                    ps = P.ps(npz % 4)
                    npz += 1
                    for tt in range(4):
                        P.mm(ps[:, tt * 128:(tt + 1) * 128], XMB[ft][:, tt * 128:(tt + 1) * 128], BD[2][:, ft, :])
                    vst = VST[ft % 2]
                    P.copy("dve", vst, ps.re("p (a f) -> p a f", f=128))
                    P.dma("sp", self.dv(scr["VTK"][ct * CT:(ct + 1) * CT, ft * 128:(ft + 1) * 128].rearrange("(t p) f -> p t f", p=128), "mlVTK", ct), vst)
                cs = slice(c * CT, (c + 1) * CT)
                P.act(GI[0:8, cs], psGI[0:8], AF.Identity, bias=BG[:, 0:1])
                P.act(LF[0:8, cs], psGF[0:8], AF.Exp, bias=BG[:, 2:3], scale=-1.0)
                P.act(LF[0:8, cs], LF[0:8, cs], AF.Ln, bias=1.0)
            P.release(m1)
            P.ts("dve", LF[0:8], LF[0:8], -1.0, ALU.mult)
            P.scan(BT[0:8], self.onec[0:8, 0:1].bc([8, S]), LF[0:8], 0.0, ALU.mult, ALU.add)
            P.tt("dve", GI[0:8], GI[0:8], BT[0:8], ALU.subtract)
            pt = P.ps(5)
            for t in range(NTT):
                P.tr(pt[:, t * 8:(t + 1) * 8], GI[0:8, t * 128:(t + 1) * 128], self.ident[0:8, 0:8])
            P.copy("dve", CTK, pt[:, 0:NTT * 8].re("p (t e) -> p t e", e=8))
            GOUT = [P.alloc([S], BF16) for _ in range(KT)]
            m2 = P.mark()
            QH = [[P.alloc([S], BF16) for _ in range(2)] for _ in range(1)]
            KH = [[P.alloc([S], BF16) for _ in range(2)] for _ in range(1)]
            VH = [P.alloc([NTT, 256], BF16) for _ in range(1)]
            BBC = P.alloc([CT], F32)
            DM = [P.alloc([CT], F32) for _ in range(2)]
            PM = [P.alloc([CT], BF16) for _ in range(2)]
            RD = P.alloc([CT], F32)
            HS = [P.alloc([CT], F32) for _ in range(2)]
            SQ = [P.alloc([CT], F32) for _ in range(2)]
            XCt = [P.alloc([CT], F32) for _ in range(2)]
            SZt = [P.alloc([CT], F32) for _ in range(2)]
            MEAN = P.alloc([CT], F32)
            RSTD = P.alloc([CT], F32)
            t0, t1 = s * S, (s + 1) * S
            seqkeys = lambda nm: [("d", nm, s * NT + c) for c in range(NT)]
            nps = 0
            for h in range(8):
                qh, kh, vh = QH[0], KH[0], VH[0]
                for d in range(2):
                    f = 2 * h + d
                    P.dma("sp", qh[d], View(scr["QT"][f * 128:(f + 1) * 128, t0:t1], seqkeys("mlQT")))
                    P.dma("sp", kh[d], View(scr["KTs"][f * 128:(f + 1) * 128, t0:t1], seqkeys("mlKTs")))
                P.dma("sp", vh, View(scr["VTK"][t0:t1, h * 256:(h + 1) * 256].rearrange("(t p) f -> p t f", p=128), seqkeys("mlVTK")))
                for tq in range(NT):
                    ct = s * NT + tq
                    ts_ = slice(tq * CT, (tq + 1) * CT)
                    pb = P.ps(5)
                    P.mm(pb, self.SELH[0:8, h, :], BT[0:8, ts_])
                    P.copy("act", BBC, pb)
                    pn0, pn1, pd = P.ps(2), P.ps(3), P.ps(4)
                    ns = 4 * (tq + 1)
                    for si in range(ns):
                        pS = P.ps(nps % 2)
                        dm, pm = DM[nps % 2], PM[nps % 2]
                        nps += 1
                        ss = slice(si * 128, (si + 1) * 128)
                        P.mm(pS, kh[0][:, ss], qh[0][:, ts_], start=True, stop=False)
                        P.mm(pS, kh[1][:, ss], qh[1][:, ts_], start=False, stop=True)
                        cst = CTK[:, si, h:h + 1]
                        if si >= 4 * tq:
                            P.stt("dve", dm, BBC, cst, MASK[:, si - 4 * tq, :], ALU.add, ALU.add)
                            P.act(dm, dm, AF.Exp)
                        else:
                            P.act(dm, BBC, AF.Exp, bias=cst)
                        P.stt("dve", pm, pS, 1.0 / 16.0, dm, ALU.mult, ALU.mult)
                        P.mm(pn0, vh[:, si, 0:128], pm, start=(si == 0), stop=(si == ns - 1))
                        P.mm(pn1, vh[:, si, 128:256], pm, start=(si == 0), stop=(si == ns - 1))
                        P.mm(pd, self.ONESB, pm, start=(si == 0), stop=(si == ns - 1))
                    P.act(RD, pd, AF.Abs)
                    P.ts("dve", RD, RD, 1.0, ALU.max)
                    P.recip(RD, RD)
                    P.tt("dve", HS[0], pn0, RD, ALU.mult)
                    P.tt("dve", HS[1], pn1, RD, ALU.mult)
                    pm_, pe_ = P.ps(6), P.ps(7)
                    for d in range(2):
                        P.mm(pm_, self.ONES256, HS[d], start=(d == 0), stop=(d == 1))
                    for d in range(2):
                        P.act(SQ[d], HS[d], AF.Square)
                        P.mm(pe_, self.ONES256, SQ[d], start=(d == 0), stop=(d == 1))
                    P.copy("act", MEAN, pm_)
                    P.tt("pool", SQ[0], MEAN, MEAN, ALU.mult)
                    P.tt("dve", RSTD, pe_, SQ[0], ALU.subtract)
                    P.act(RSTD, RSTD, AF.Sqrt, bias=self.epsc[:, 1:2])
                    P.recip(RSTD, RSTD)
                    for d in range(2):
                        f = 2 * h + d
                        P.dma("sp", XCt[d], self.dv(scr["XC"][f * 128:(f + 1) * 128, ct * CT:(ct + 1) * CT], "mlXC", ct))
                        P.dma("sp", SZt[d], self.dv(scr["SZ"][f * 128:(f + 1) * 128, ct * CT:(ct + 1) * CT], "mlSZ", ct))
                        P.tt("dve", HS[d], HS[d], MEAN, ALU.subtract)
                        P.tt("pool", HS[d], HS[d], RSTD, ALU.mult)
                        P.ts("pool", HS[d], HS[d], VP[:, 6, f:f + 1], ALU.mult)
                        P.stt("dve", HS[d], XCt[d], VP[:, 5, f:f + 1], HS[d], ALU.mult, ALU.add)
                        P.tt("pool", GOUT[f][:, ts_], HS[d], SZt[d], ALU.mult)
            P.release(m2)
            X = [P.alloc([CT], F32) for _ in range(KT)]
            WB = [P.alloc([KT, CT], BF16) for _ in range(2)]
            for c in range(NT):
                ct = s * NT + c
                ts_ = slice(c * CT, (c + 1) * CT)
                self.load_x_tiles(xsrc, xname, ct, X)
                def body_o(nb, w):
                    nonlocal npz
                    for mi in range(4):
                        ft = nb * 4 + mi
                        ps = P.ps(npz % 4)
                        npz += 1
                        for k in range(KT):
                            P.mm(ps, w[:, k, mi * 128:(mi + 1) * 128], GOUT[k][:, ts_], start=(k == 0), stop=(k == KT - 1))
                        P.stt("dve", X[ft], ps, self.mod(li, 2, ft, s), X[ft], ALU.mult, ALU.add)
                self.stream(WB, [(self.ml_w_out[j][:, nb * CT:(nb + 1) * CT], KT, "ml_w_out") for nb in range(4)], body_o)
                self.resid_ln(li, 1, ct, X, self.X1, "X1")
        P.release(m0)


def _pp(v):
    v = np.asarray(v, np.float32)
    sh = v.shape
    v = v.reshape(sh[:-1] + (sh[-1] // 128, 128))
    return np.ascontiguousarray(np.moveaxis(v, -1, 0))


def rg_slab_tiles(j):
    b0, b1 = (128 * j) // 160, (128 * j + 127) // 160
    i0, i1 = (160 * b0) // 128, (160 * b1 + 159) // 128
    return list(range(i0, i1 + 1))


class _Sel:
    def __init__(self, inp, sel):
        self.inp, self.sel = inp, sel

    def __getitem__(self, name):
        return _SelArr(self.inp[name], self.sel[name])


class _SelArr:
    def __init__(self, a, idx):
        self.a, self.idx = a, idx

    def __getitem__(self, key):
        if isinstance(key, (int, np.integer)):
            return np.asarray(self.a[self.idx[key]], np.float32)
        g = np.stack([np.asarray(self.a[i], np.float32) for i in self.idx], axis=0)
        if isinstance(key, tuple):
            return g[(slice(None),) + key[1:]]
        return g


def prep_shared(inp_raw, layers, li_ids=None):
    L = len(layers)
    li_ids = list(range(L)) if li_ids is None else list(li_ids)
    jsel = {k: [j for kk, j in layers if kk == k] or [0] for k in (0, 1, 2)}
    sel = {}
    for name in inp_raw:
        if name.startswith("rg_"):
            sel[name] = jsel[0]
        elif name.startswith("sg_"):
            sel[name] = jsel[1]
        elif name.startswith("ml_"):
            sel[name] = jsel[2]
        else:
            sel[name] = li_ids
    inp = _Sel(inp_raw, sel)
    out = {}
    f = lambda a: np.ascontiguousarray(np.asarray(a, np.float32))
    out["ada_w"] = f(inp["ada_w"][:L])
    out["ada_bT"] = _pp(inp["ada_b"][:L])
    lnp = np.stack([inp["ln1_g"][:L], inp["ln1_b"][:L], inp["ln2_g"][:L], inp["ln2_b"][:L]], axis=1)
    out["lnp"] = _pp(lnp)
    out["ident"] = np.eye(128, dtype=np.float32)
    mk = np.zeros((128, 4, 512), np.float32)
    for o in range(4):
        sidx = o * 128 + np.arange(128)[:, None]
        tidx = np.arange(512)[None, :]
        mk[:, o, :] = np.where(sidx <= tidx, 0.0, -30000.0)
    out["maskneg"] = mk
    nA = max(1, sum(1 for k, _ in layers if k == 0))
    nB = max(1, sum(1 for k, _ in layers if k == 1))
    nC = max(1, sum(1 for k, _ in layers if k == 2))
    out["rg_w_in"] = f(inp["rg_w_in"][:nA])
    vec = np.concatenate([inp["rg_conv_w"][:nA], inp["rg_conv_b"][:nA, None], inp["rg_b_rgate"][:nA, None],
                          inp["rg_b_igate"][:nA, None], inp["rg_lam"][:nA, None], np.zeros_like(inp["rg_lam"][:nA, None])], axis=1)
    out["rg_vec"] = _pp(vec)
    slabs = np.zeros((nA, RGT, 128, 2, 4, 128), np.float32)
    for a in range(nA):
        for gi, wname in enumerate(("rg_w_rgate", "rg_w_igate")):
            w = np.asarray(inp[wname][a], np.float32)
            for jt in range(RGT):
                for si, it in enumerate(rg_slab_tiles(jt)):
                    for n in range(16):
                        r0, r1 = max(it * 128, n * 160), min(it * 128 + 128, n * 160 + 160)
                        c0, c1 = max(jt * 128, n * 160), min(jt * 128 + 128, n * 160 + 160)
                        if r0 < r1 and c0 < c1:
                            slabs[a, jt, r0 - it * 128:r1 - it * 128, gi, si, c0 - jt * 128:c1 - jt * 128] = \
                                w[n, r0 - n * 160:r1 - n * 160, c0 - n * 160:c1 - n * 160]
    out["rg_slabs"] = slabs
    out["rg_w_out"] = f(inp["rg_w_out"][:nA])
    out["sg_w_in"] = f(inp["sg_w_in"][:nB])
    out["sg_vecp"] = _pp(np.stack([inp["sg_b_in"][:nB, :D], inp["sg_b_out"][:nB]], axis=1))
    bsp = np.zeros((nB, D), np.float32)
    bsp[:, :1024] = np.asarray(inp["sg_b_sp"][:nB]).reshape(nB, 1024)
    out["sg_rows"] = f(np.stack([inp["sg_b_in"][:nB, D:], inp["sg_ln_g"][:nB], inp["sg_ln_b"][:nB], bsp], axis=1))
    out["sg_w_spT"] = f(np.transpose(np.asarray(inp["sg_w_sp"][:nB]), (0, 3, 1, 2)))
    out["sg_w_out"] = f(inp["sg_w_out"][:nB])
    out["ml_w_in"] = f(inp["ml_w_in"][:nC])
    out["ml_vecp"] = _pp(np.concatenate([inp["ml_conv_w"][:nC], inp["ml_conv_b"][:nC, None], inp["ml_skip"][:nC, None],
                                         inp["ml_norm_g"][:nC, None]], axis=1))
    bd = np.zeros((nC, 3, 128, KT, 128), np.float32)
    for a in range(nC):
        for qi, wname in enumerate(("ml_w_q", "ml_w_k", "ml_w_v")):
            w = np.asarray(inp[wname][a], np.float32)
            for t in range(KT):
                for b in range(32):
                    bd[a, qi, b * 4:b * 4 + 4, t, b * 4:b * 4 + 4] = w[t * 32 + b]
    out["ml_bd"] = bd
    wg = np.asarray(inp["ml_w_gates"][:nC], np.float32).reshape(nC, 48, 128, 16)
    out["ml_w_gates"] = f(np.transpose(wg, (0, 2, 1, 3)))
    out["ml_b_gates"] = f(np.transpose(np.asarray(inp["ml_b_gates"][:nC]).reshape(nC, 2, 8), (2, 0, 1)))
    out["ml_w_out"] = f(inp["ml_w_out"][:nC])
    wr = np.asarray(inp["moe_w_router"][:L], np.float32).reshape(L, KT, 128, NE)
    out["moe_w_router"] = f(np.transpose(wr, (0, 2, 1, 3)))
    out["moe_b_router"] = f(np.asarray(inp["moe_b_router"][:L]).reshape(L, NE, 1))
    out["moe_w_gate_up"] = f(inp["moe_w_gate_up"][:L])
    out["moe_b_gu"] = _pp(inp["moe_b_gate_up"][:L])
    out["moe_w_down"] = f(inp["moe_w_down"][:L])
    out["moe_b_down"] = f(inp["moe_b_down"][:L])
    return out


def prep_core(x, c, b0, nseq):
    xs = np.asarray(x[b0:b0 + nseq], np.float32)
    S = xs.shape[1]
    xT = np.ascontiguousarray(xs.reshape(nseq * S, D).T)
    cT = _pp(np.asarray(c[b0:b0 + nseq], np.float32))
    cT = np.ascontiguousarray(np.transpose(cT, (0, 2, 1)))
    return {"xT": xT, "cT": cT}


_CACHE = {}


def run(inp, layers, nseq, ncores, trace=False, li_ids=None):
    x = np.asarray(inp["x"])
    B, S, _ = x.shape
    cnt = {0: 0, 1: 0, 2: 0}
    local = []
    for k, j in layers:
        local.append((k, cnt[k]))
        cnt[k] += 1
    key = (S, nseq, tuple(local))
    if key not in _CACHE:
        _CACHE[key] = MK(NSEQ=nseq, S=S, layers=local)
    mk = _CACHE[key]
    shared = prep_shared(inp, layers, li_ids)
    in_maps = []
    for r in range(ncores):
        d = dict(shared)
        d.update(prep_core(x, inp["c"], r * nseq, nseq))
        for n, (shape, _) in mk.inputs.items():
            assert tuple(d[n].shape) == shape, (n, d[n].shape, shape)
        in_maps.append({n: d[n] for n in mk.inputs})
    res = run_bass_kernel_spmd(mk.nc, in_maps, core_ids=list(range(ncores)), trace=trace)
    outs = []
    for r in range(ncores):
        oT = np.asarray(res.results[r]["outT"])
        outs.append(oT.T.reshape(nseq, S, D))
    return np.concatenate(outs, axis=0), res


LAUNCHES = [([(0, 0), (1, 0)], [0, 1]), ([(2, 0), (0, 1)], [2, 3])]


def kernel(**inputs):
    inp = dict(inputs)
    out = None
    for layers, li_ids in LAUNCHES:
        out, _ = run(inp, layers, nseq=2, ncores=8, li_ids=li_ids)
        inp["x"] = out
    return out.astype(np.float32)
```
